# Optimizing a Trainium2 kernel written in Bass

```python
import math
import jax, jax.numpy as jnp
from jax import lax
import numpy as np

D_MODEL = 1024
BATCH = 32
SEQ = 2048
DEPTH = 1

MIX_WIDTH = D_MODEL
ATTN_WIDTH = MIX_WIDTH // 2
CONV_WIDTH = MIX_WIDTH - ATTN_WIDTH
HEAD_DIM = 64
N_HEADS = ATTN_WIDTH // HEAD_DIM
DILATED_PATTERNS = ((128, 1), (512, 4), (2048, 16))
CONV_KERNEL = 31
N_EXPERTS = 16
CAPACITY_FACTOR = 2
D_FF_EXPERT = 2816
ROPE_THETA = 10000.0
EPS = 1e-6
NEG_INF = -1e30
IN_COLS = 3 * ATTN_WIDTH + 2 * CONV_WIDTH

kernel_name = "hybrid_dilated_attn_conformer_ec_moe"


def rms_norm(x, g):
    xf = x.astype(jnp.float32)
    y = xf * lax.rsqrt(jnp.mean(xf * xf, axis=-1, keepdims=True) + EPS)
    return (y * g.astype(jnp.float32)).astype(x.dtype)


def layer_norm(x, g, b):
    xf = x.astype(jnp.float32)
    mu = jnp.mean(xf, axis=-1, keepdims=True)
    xc = xf - mu
    y = xc * lax.rsqrt(jnp.mean(xc * xc, axis=-1, keepdims=True) + EPS)
    return (y * g.astype(jnp.float32) + b.astype(jnp.float32)).astype(x.dtype)


def rope(x, positions):
    half = x.shape[-1] // 2
    inv_freq = ROPE_THETA ** (-jnp.arange(half, dtype=jnp.float32) / half)
    ang = positions.astype(jnp.float32)[:, None] * inv_freq[None, :]
    cos, sin = jnp.cos(ang), jnp.sin(ang)
    xf = x.astype(jnp.float32)
    x1, x2 = xf[..., :half], xf[..., half:]
    out = jnp.concatenate([x1 * cos - x2 * sin, x2 * cos + x1 * sin], axis=-1)
    return out.astype(x.dtype)


def banded_attention(q, k, v, n):
    L, hd = q.shape[-2], q.shape[-1]
    lead = q.shape[:-2]
    nb = -(-L // n)
    Lp = nb * n
    lp = [(0, 0)] * len(lead)
    qb = jnp.pad(q, lp + [(0, Lp - L), (0, 0)]).reshape(*lead, nb, n, hd)

    def windows(t):
        tp = jnp.pad(t, lp + [(n, Lp - L + n), (0, 0)]).reshape(*lead, nb + 2, n, hd)
        return jnp.concatenate([tp[..., 0:nb, :, :], tp[..., 1:nb + 1, :, :],
                                tp[..., 2:nb + 2, :, :]], axis=-2)

    kw, vw = windows(k), windows(v)
    j = jnp.arange(nb)[:, None, None]
    qpos = j * n + jnp.arange(n)[None, :, None]
    kpos = j * n - n + jnp.arange(3 * n)[None, None, :]
    mask = (jnp.abs(qpos - kpos) <= n) & (kpos >= 0) & (kpos < L)
    s = jnp.einsum('...jqd,...jkd->...jqk', qb.astype(jnp.float32),
                   kw.astype(jnp.float32)) * (hd ** -0.5)
    s = jnp.where(mask, s, NEG_INF)
    m = jnp.max(s, axis=-1, keepdims=True)
    p = jnp.exp(s - m)
    l = jnp.sum(p, axis=-1, keepdims=True)
    o = jnp.einsum('...jqk,...jkd->...jqd', p, vw.astype(jnp.float32)) / l
    lse = (m + jnp.log(l))[..., 0]
    o = o.reshape(*lead, Lp, hd)[..., :L, :]
    lse = lse.reshape(*lead, Lp)[..., :L]
    return o, lse


def dilated_attention(q, k, v, window, dilation):
    B, H, S, hd = q.shape
    L = S // dilation

    def split(t):
        return t.reshape(B, H, L, dilation, hd).swapaxes(2, 3)

    o, lse = banded_attention(split(q), split(k), split(v), window // (2 * dilation))
    o = o.swapaxes(2, 3).reshape(B, H, S, hd)
    lse = lse.swapaxes(2, 3).reshape(B, H, S)
    return o, lse


def conformer_conv(a, b, dw_w, dw_b, ln_g, ln_b):
    u = a * jax.nn.sigmoid(b)
    C = u.shape[-1]
    pad = CONV_KERNEL // 2
    u = lax.conv_general_dilated(u, dw_w[:, None, :], window_strides=(1,),
                                 padding=[(pad, pad)],
                                 dimension_numbers=('NWC', 'WIO', 'NWC'),
                                 feature_group_count=C) + dw_b
    u = layer_norm(u, ln_g, ln_b)
    return jax.nn.silu(u)


def expert_choice_moe(h, w_router, w1, w3, w2):
    B, S, D = h.shape
    aff = jax.nn.softmax(jnp.einsum('bsd,de->bse', h, w_router).astype(jnp.float32), axis=-1)
    cap = CAPACITY_FACTOR * S // N_EXPERTS
    gate, idx = lax.top_k(aff.swapaxes(1, 2), cap)
    xe = jax.vmap(lambda hb, ib: hb[ib])(h, idx)

    def expert(args):
        xt, w1e, w3e, w2e = args
        return (jax.nn.silu(xt @ w1e) * (xt @ w3e)) @ w2e

    ye = lax.map(expert, (xe.swapaxes(0, 1), w1, w3, w2))
    ye = ye.swapaxes(0, 1) * gate[..., None].astype(ye.dtype)
    ye = ye.reshape(B, N_EXPERTS * cap, D)
    return jax.vmap(lambda y, i: jnp.zeros((S, D), y.dtype).at[i].add(y))(
        ye, idx.reshape(B, N_EXPERTS * cap))


def setup_inputs(seed: int = 0) -> dict:
    key = jax.random.key(seed)
    ks = jax.random.split(key, 16)
    f32 = jnp.float32
    nrm = lambda k, shape, scale: jax.random.normal(k, shape, f32) * scale
    return {
        "x": jax.random.normal(ks[0], (BATCH, SEQ, D_MODEL), f32),
        "norm1_g": 1.0 + nrm(ks[1], (D_MODEL,), 0.02),
        "w_in": nrm(ks[2], (D_MODEL, IN_COLS), D_MODEL ** -0.5),
        "q_norm_g": 1.0 + nrm(ks[3], (HEAD_DIM,), 0.02),
        "k_norm_g": 1.0 + nrm(ks[4], (HEAD_DIM,), 0.02),
        "conv_dw_w": nrm(ks[5], (CONV_KERNEL, CONV_WIDTH), CONV_KERNEL ** -0.5),
        "conv_dw_b": nrm(ks[6], (CONV_WIDTH,), 0.01),
        "conv_ln_g": 1.0 + nrm(ks[7], (CONV_WIDTH,), 0.02),
        "conv_ln_b": nrm(ks[8], (CONV_WIDTH,), 0.01),
        "w_out": nrm(ks[9], (MIX_WIDTH, D_MODEL), MIX_WIDTH ** -0.5),
        "norm2_g": 1.0 + nrm(ks[10], (D_MODEL,), 0.02),
        "w_router": nrm(ks[11], (D_MODEL, N_EXPERTS), D_MODEL ** -0.5),
        "w1": nrm(ks[12], (N_EXPERTS, D_MODEL, D_FF_EXPERT), D_MODEL ** -0.5),
        "w3": nrm(ks[13], (N_EXPERTS, D_MODEL, D_FF_EXPERT), D_MODEL ** -0.5),
        "w2": nrm(ks[14], (N_EXPERTS, D_FF_EXPERT, D_MODEL), D_FF_EXPERT ** -0.5),
    }


def reference(x, norm1_g, w_in, q_norm_g, k_norm_g, conv_dw_w, conv_dw_b,
              conv_ln_g, conv_ln_b, w_out, norm2_g, w_router, w1, w3, w2):
    B, S, D = x.shape
    positions = jnp.arange(S)
    for _ in range(DEPTH):
        h = rms_norm(x, norm1_g)
        z = jnp.einsum('bsd,dc->bsc', h, w_in)
        A = ATTN_WIDTH

        def heads(t):
            return t.reshape(B, S, N_HEADS, HEAD_DIM).transpose(0, 2, 1, 3)

        q = rope(rms_norm(heads(z[..., 0:A]), q_norm_g), positions)
        k = rope(rms_norm(heads(z[..., A:2 * A]), k_norm_g), positions)
        v = heads(z[..., 2 * A:3 * A])
        outs, lses = [], []
        for window, dilation in DILATED_PATTERNS:
            o_p, lse_p = dilated_attention(q, k, v, window, dilation)
            outs.append(o_p)
            lses.append(lse_p)
        w_mix = jax.nn.softmax(jnp.stack(lses), axis=0)
        attn = jnp.einsum('pbhs,pbhsd->bhsd', w_mix, jnp.stack(outs))
        attn = attn.transpose(0, 2, 1, 3).reshape(B, S, A).astype(x.dtype)

        c0 = 3 * A
        conv = conformer_conv(z[..., c0:c0 + CONV_WIDTH],
                              z[..., c0 + CONV_WIDTH:c0 + 2 * CONV_WIDTH],
                              conv_dw_w, conv_dw_b, conv_ln_g, conv_ln_b)
        mixed = jnp.concatenate([attn, conv.astype(x.dtype)], axis=-1)
        x = x + jnp.einsum('bsc,cd->bsd', mixed, w_out)

        x = x + expert_choice_moe(rms_norm(x, norm2_g), w_router, w1, w3, w2)
    return x
```

```python
import numpy as np
from contextlib import ExitStack
import ml_dtypes
import concourse.bass as bass
import concourse.mybir as mybir
from concourse.bass_utils import run_bass_kernel_spmd

F32 = mybir.dt.float32
BF16 = mybir.dt.bfloat16
I32 = mybir.dt.int32
U32 = mybir.dt.uint32
AF = mybir.ActivationFunctionType
ALU = mybir.AluOpType
AX = mybir.AxisListType

ENGS = ("pe", "act", "dve", "pool", "sp")

D = 1024
S = 2048
NT = 16
HD = 64
NH = 8
CW = 512
KC = 31
NE = 16
CAP = 256
DFF = 2816
NFT = DFF // 128
EPS = 1e-6
MOFF = 1920
MU = 3968
N_CORES = 8
NWARM = 0


_UID = [0]
LIMIT = [None]


def _u(name):
    _UID[0] += 1
    return f"{name}_{_UID[0]}"


class SemPool:
    def __init__(self, nc, stack):
        self.nc = nc
        self.stack = stack
        self.esem = {e: stack.enter_context(nc.semaphore(f"s_{e}")) for e in ENGS}
        self.ecount = {e: 0 for e in ENGS}
        self.dsem = {}
        self.dcount = {}
        self.nblocks = 0

    def stream(self, s):
        if s not in self.dsem:
            self.dsem[s] = self.stack.enter_context(self.nc.semaphore(f"d_{s}"))
            self.dcount[s] = 0
        return self.dsem[s]


class Sched:
    def __init__(self, nc, name):
        self.nc = nc
        self.name = name
        self.ops = {e: [] for e in ENGS}
        self.last_w = {}
        self.readers = {}
        self.streams = {}
        self.stream_order = []

    def _deps(self, reads, writes):
        deps = []
        for r in reads:
            t = self.last_w.get(r)
            if t is not None:
                deps.append(t)
        for w in writes:
            t = self.last_w.get(w)
            if t is not None:
                deps.append(t)
            deps.extend(self.readers.get(w, ()))
        return deps

    def _commit(self, tok, reads, writes):
        for r in reads:
            self.readers.setdefault(r, []).append(tok)
        for w in writes:
            self.last_w[w] = tok
            self.readers[w] = []

    def op(self, eng, fn, reads=(), writes=()):
        deps = self._deps(reads, writes)
        tok = ("e", eng, len(self.ops[eng]))
        self.ops[eng].append(dict(fn=fn, deps=deps, ms=False, dma=None))
        self._commit(tok, reads, writes)
        return tok

    def dma(self, q, fn, stream, reads=(), writes=()):
        deps = self._deps(reads, writes)
        if stream not in self.streams:
            self.streams[stream] = 0
            self.stream_order.append(stream)
        self.streams[stream] += 1
        tok = ("d", stream, self.streams[stream])
        self.ops[q].append(dict(fn=fn, deps=deps, ms=False, dma=stream))
        self._commit(tok, reads, writes)
        return tok

    def wait_all(self, eng, keys):
        self.op(eng, None, reads=list(keys), writes=list(keys))

    def emit(self, stack, pool):
        nc = self.nc
        pool.nblocks += 1
        if LIMIT[0] is not None and pool.nblocks > LIMIT[0]:
            return
        for e in ENGS:
            for rec in self.ops[e]:
                for d in rec["deps"]:
                    if d[0] == "e" and not (d[1] == "pe" and e == "pe"):
                        self.ops[d[1]][d[2]]["ms"] = True
        msval = {}
        nms = {}
        for e in ENGS:
            c = pool.ecount[e]
            for i, rec in enumerate(self.ops[e]):
                if rec["ms"]:
                    c += 1
                    msval[(e, i)] = c
            nms[e] = c
        esem = pool.esem
        dsem = {s: pool.stream(s) for s in self.stream_order}
        dbase = {s: pool.dcount[s] for s in self.stream_order}
        block = stack.enter_context(nc.Block())
        engobj = {"pe": block.tensor, "act": block.scalar, "dve": block.vector,
                  "pool": block.gpsimd, "sp": block.sync}

        def run(e):
            def body(eng):
                waited = {}
                for rec in self.ops[e]:
                    need = {}
                    for d in rec["deps"]:
                        if d[0] == "e":
                            if d[1] == "pe" and e == "pe":
                                continue
                            key = ("e", d[1])
                            val = msval[(d[1], d[2])]
                        else:
                            key = ("d", d[1])
                            val = 16 * (dbase[d[1]] + d[2])
                        if val > need.get(key, 0):
                            need[key] = val
                    for key, val in need.items():
                        if waited.get(key, 0) >= val:
                            continue
                        waited[key] = val
                        sem = esem[key[1]] if key[0] == "e" else dsem[key[1]]
                        eng.wait_ge(sem, val)
                    if rec["fn"] is None:
                        if rec["ms"]:
                            eng.nop().then_inc(esem[e], 1)
                        continue
                    ins = rec["fn"](eng)
                    if rec["dma"] is not None:
                        ins.then_inc(dsem[rec["dma"]], 16)
                    elif rec["ms"]:
                        ins.then_inc(esem[e], 1)
                if e == "sp":
                    for s in self.stream_order:
                        eng.wait_ge(dsem[s], 16 * (dbase[s] + self.streams[s]))
            return body

        for e in ENGS:
            engobj[e](run(e))
        for e in ENGS:
            pool.ecount[e] = nms[e]
        for s in self.stream_order:
            pool.dcount[s] += self.streams[s]


def _count(d):
    ad = abs(d)
    c = 0
    if ad <= 64:
        c += 1
    if d % 4 == 0 and ad <= 256:
        c += 1
    if d % 16 == 0 and ad <= 1024:
        c += 1
    return c


def host_consts(NB):
    half = HD // 2
    inv_freq = (10000.0 ** (-np.arange(half, dtype=np.float32) / half)).astype(np.float32)
    pos = np.arange(S, dtype=np.float32)
    ang = pos[:, None] * inv_freq[None, :]
    cos = np.cos(ang).astype(np.float32).reshape(NT, 128, half).transpose(1, 0, 2)
    sin = np.sin(ang).astype(np.float32).reshape(NT, 128, half).transpose(1, 0, 2)
    cvals = np.array([_count(d) for d in range(-(MU + 128), MU + 128)], dtype=np.float32)
    u = np.arange(MU)[None, :]
    p = np.arange(128)[:, None]
    dd = u - p - MOFF
    maskT = cvals[dd + (MU + 128)].astype(ml_dtypes.bfloat16)
    offs = (np.arange(NB * NE) // NE * S).astype(np.float32).reshape(NB * NE, 1)
    return dict(
        c_cos=np.ascontiguousarray(cos), c_sin=np.ascontiguousarray(sin),
        c_mask=np.ascontiguousarray(maskT),
        c_ident=np.eye(128, dtype=np.float32),
        c_offs=offs,
    )


def build_nc(NB):
    nc = bass.Bass("TRN2", target_bir_lowering=False)
    NTOK = NB * S
    NP = NB * NE

    def din(name, shape, dt=F32):
        return nc.dram_tensor(name, list(shape), dt, kind="ExternalInput").ap()

    x = din("x", [NTOK, D])
    g1b = din("g1b", [128, D])
    g2b = din("g2b", [128, D])
    w_in = din("w_in", [D, 2560])
    qgb = din("qgb", [128, HD])
    kgb = din("kgb", [128, HD])
    cw = din("cw", [128, 4, KC])
    cb = din("cb", [128, 4])
    lg = din("lg", [128, 4])
    lb = din("lb", [128, 4])
    w_out = din("w_out", [D, D])
    w_router = din("w_router", [D, NE])
    w1 = din("w1", [NE, D, DFF])
    w3 = din("w3", [NE, D, DFF])
    w2 = din("w2", [NE, DFF, D])
    c_cos = din("c_cos", [128, NT, 32])
    c_sin = din("c_sin", [128, NT, 32])
    c_mask = din("c_mask", [128, MU], BF16)
    c_ident = din("c_ident", [128, 128])
    c_offs = din("c_offs", [NP, 1])
    y = nc.dram_tensor("y", [NTOK, D], F32, kind="ExternalOutput").ap()
    h2s = nc.dram_tensor("h2s", [NTOK, D], BF16, kind="Internal").ap()

    outer = ExitStack()
    with outer:
        pool = SemPool(nc, outer)

        def sbo(name, shape, dt):
            return outer.enter_context(nc.sbuf_tensor(_u(name), list(shape), dt))

        idxT = sbo("idxT", [128, 2, NP], I32)
        gateT = sbo("gateT", [128, 2, NP], F32)
        ident_bf = sbo("ident_bf", [128, 128], BF16)
        ident_f = sbo("ident_f", [128, 128], F32)
        ones_f = sbo("ones_f", [128, 128], F32)
        aff_all = sbo("aff_all", [128, NT, NB, NE], F32)

        def rstd_chain(op, ss_ap, out_ap, n, keyin, keyout):
            op("dve", lambda e: e.tensor_scalar(out=out_ap, in0=ss_ap, scalar1=1.0 / n, scalar2=EPS,
                                                op0=ALU.mult, op1=ALU.add), reads=[keyin], writes=[keyout])
            op("act", lambda e: e.sqrt(out=out_ap, in_=out_ap), reads=[keyout], writes=[keyout])
            op("dve", lambda e: e.reciprocal(out=out_ap, in_=out_ap), reads=[keyout], writes=[keyout])

        stSeq = ExitStack()
        with stSeq:
            def sbq(name, shape, dt):
                return stSeq.enter_context(nc.sbuf_tensor(_u(name), list(shape), dt))
            qT = sbq("qT", [128, 4, S], BF16)
            kTp = sbq("kTp", [128, NH, S], BF16)
            v1f = sbq("v1f", [128, NT * NH * 65 + 64], BF16)
            v1 = v1f[:, 0:NT * NH * 65].rearrange("p (t h d) -> p t h d", h=NH, d=65)
            u_sb = sbq("u_sb", [128, 4, S + 30], BF16)
            convT = sbq("convT", [128, 4, S], BF16)

            st = ExitStack()
            with st:
                I = Sched(nc, "I")
                I.dma("sp", lambda e: e.dma_start(out=ident_f[:], in_=c_ident), "c0", writes=["ident_f"])
                I.op("dve", lambda e: e.tensor_copy(out=ident_bf[:], in_=ident_f[:]), reads=["ident_f"], writes=["ident_bf"])
                I.op("pool", lambda e: e.memset(ones_f[:], 1.0), writes=["ones_f"])
                I.op("pool", lambda e: e.memset(v1f[:], 1.0), writes=["v1_ones"])
                I.op("pool", lambda e: e.memset(kTp[:], 0.0), writes=["kTp_zero"])
                I.op("pool", lambda e: e.memset(u_sb[:], 0.0), writes=["u_zero"])
                I.emit(st, pool)

            for b in range(NB):
                r0 = b * S
                st = ExitStack()
                with st:
                    def sb(name, shape, dt):
                        return st.enter_context(nc.sbuf_tensor(_u(name), list(shape), dt))

                    def ps(name, shape, dt):
                        return st.enter_context(nc.psum_tensor(_u(name), list(shape), dt))
                    win_sb = sb("win_sb", [128, 8, 2560], BF16)
                    g1_sb = sb("g1_sb", [128, D], F32)
                    qg_sb = sb("qg_sb", [128, HD], F32)
                    kg_sb = sb("kg_sb", [128, HD], F32)
                    cos_sb = sb("cos_sb", [128, NT, 32], F32)
                    sin_sb = sb("sin_sb", [128, NT, 32], F32)
                    xt = [sb(f"xt{i}", [128, D], F32) for i in range(2)]
                    junk = sb("junk", [128, D], BF16)
                    hb = [sb(f"hb{i}", [128, D], BF16) for i in range(2)]
                    hT = [sb(f"hT{i}", [128, 8, 512], BF16) for i in range(2)]
                    st_ss = sb("st_ss", [128, 4], F32)
                    rstd_all = sb("rstd_all", [128, NT], F32)
                    sqt2 = [sb(f"sqt{g}", [128, 512], F32) for g in range(2)]
                    ssq = sb("ssq", [128, 2, NH], F32)
                    qg_t2 = [sb(f"qg_t{g}", [128, 512], F32) for g in range(2)]
                    rt2 = [[sb(f"rt{g}_{i}", [128, NH, 32], F32) for i in range(4)] for g in range(2)]
                    ro2 = [sb(f"ro{g}", [128, NH, HD], F32) for g in range(2)]
                    qn = [sb(f"qn{i}", [128, 512], BF16) for i in range(2)]
                    sg_sb = sb("sg_sb", [128, 512], F32)
                    banks = [ps(f"bank{i}", [128, 512], F32) for i in range(6)]
                    bankb = [ps(f"bankb{i}", [128, 1024], BF16) for i in range(2)]

                    A = Sched(nc, f"P1_{b}")
                    op, dma = A.op, A.dma
                    dma("sp", lambda e: e.dma_start(out=g1_sb[:], in_=g1b), "c0", writes=["g1"])
                    dma("sp", lambda e: e.dma_start(out=qg_sb[:], in_=qgb), "c1", writes=["qg"])
                    dma("sp", lambda e: e.dma_start(out=kg_sb[:], in_=kgb), "c2", writes=["kg"])
                    dma("sp", lambda e: e.dma_start(out=cos_sb[:], in_=c_cos), "c3", writes=["cos"])
                    dma("sp", lambda e: e.dma_start(out=sin_sb[:], in_=c_sin), "c4", writes=["sin"])
                    win_v = w_in.rearrange("(kt p) c -> p kt c", p=128)
                    for c0 in range(0, 2560, 512):
                        dma("pool", lambda e, c0=c0: e.dma_start(out=win_sb[:, :, c0:c0 + 512], in_=win_v[:, :, c0:c0 + 512]),
                            f"cw{c0 // 512}", writes=[("win", c0)])
                    WQKV = [[("win", g * 512)] for g in range(3)]
                    WAB = [[("win", 1536 + ab * 512)] for ab in range(2)]

                    def front(i):
                        c, j = i // 4, i % 4
                        hs, xs, bo = c % 2, i % 2, 3 * (i % 2)
                        rows = slice(r0 + i * 128, r0 + (i + 1) * 128)
                        dma("sp", lambda e, xs=xs, rows=rows: e.dma_start(out=xt[xs][:], in_=x[rows, :]),
                            f"x{xs}", writes=[("xt", xs)])
                        op("act", lambda e, xs=xs: e.activation(out=junk[:], in_=xt[xs][:], func=AF.Square,
                                                                accum_out=st_ss[:, 0:1]),
                           reads=[("xt", xs)], writes=["junk", "ss0"])
                        rstd_chain(op, st_ss[:, 0:1], rstd_all[:, i:i + 1], D, "ss0", ("rstd", i))
                        op("dve", lambda e, xs=xs, i=i: e.scalar_tensor_tensor(
                            out=hb[xs][:], in0=xt[xs][:], scalar=rstd_all[:, i:i + 1], in1=g1_sb[:],
                            op0=ALU.mult, op1=ALU.mult),
                           reads=[("xt", xs), ("rstd", i), "g1"], writes=[("hb", xs)])

                    def front2(i):
                        c, j = i // 4, i % 4
                        hs, xs, bo = c % 2, i % 2, 3 * (i % 2)
                        for kt in range(8):
                            op("pe", lambda e, xs=xs, kt=kt: e.transpose(
                                out=bankb[0][:, kt * 128:(kt + 1) * 128], in_=hb[xs][:, kt * 128:(kt + 1) * 128],
                                identity=ident_bf[:]),
                               reads=[("hb", xs)], writes=["bb0"])
                        op("act", lambda e, hs=hs, j=j: e.copy(
                            out=hT[hs][:, :, j * 128:(j + 1) * 128],
                            in_=bankb[0][:].rearrange("p (k t) -> p k t", t=128)),
                           reads=["bb0"], writes=[("hT", hs, j)])
                        if j == 0:
                            for _ in range(NWARM):
                                op("pe", lambda e: e.matmul(banks[bo][:, :], lhsT=ident_bf[:], rhs=win_sb[:, 0, 0:512],
                                                            start=True, stop=True),
                                   reads=[("win", 0)], writes=[("bank", bo)])
                        for g in range(3):
                            for kt in range(8):
                                op("pe", lambda e, g=g, kt=kt, hs=hs, j=j: e.matmul(
                                    banks[bo + g][:, :], lhsT=hT[hs][:, kt, j * 128:(j + 1) * 128],
                                    rhs=win_sb[:, kt, g * 512:(g + 1) * 512], start=(kt == 0), stop=(kt == 7)),
                                   reads=[("hT", hs, j)] + WQKV[g], writes=[("bank", bo + g)])

                    def chain(i):
                        bo = 3 * (i % 2)
                        chains = []
                        for g, gsb, gk in ((0, qg_sb, "qg"), (1, kg_sb, "kg")):
                            steps = []
                            pq = banks[bo + g]
                            pq3 = pq[:].rearrange("p (h d) -> p h d", d=HD)
                            sqt, qg_t, rt, ro = sqt2[g], qg_t2[g], rt2[g], ro2[g]
                            reng = "dve" if g == 0 else "pool"
                            K_ = lambda s, g=g: (s, g)
                            steps.append(lambda pq=pq, sqt=sqt, g=g, bo=bo: op("act", lambda e: e.activation(out=sqt[:], in_=pq[:], func=AF.Square),
                                         reads=[("bank", bo + g)], writes=[("sqt", g)]))
                            steps.append(lambda sqt=sqt, g=g: op("dve", lambda e: e.tensor_reduce(
                                out=ssq[:, g, :], in_=sqt[:].rearrange("p (h d) -> p h d", d=HD), axis=AX.X, op=ALU.add),
                                reads=[("sqt", g)], writes=[("ssq", g)]))
                            steps.append(lambda g=g: op("dve", lambda e: e.tensor_scalar(
                                out=ssq[:, g, :], in0=ssq[:, g, :], scalar1=1.0 / HD, scalar2=EPS, op0=ALU.mult, op1=ALU.add),
                                reads=[("ssq", g)], writes=[("ssq", g)]))
                            steps.append(lambda g=g: op("act", lambda e: e.sqrt(out=ssq[:, g, :], in_=ssq[:, g, :]),
                                                        reads=[("ssq", g)], writes=[("ssq", g)]))
                            steps.append(lambda g=g: op("dve", lambda e: e.reciprocal(out=ssq[:, g, :], in_=ssq[:, g, :]),
                                                        reads=[("ssq", g)], writes=[("ssq", g)]))
                            steps.append(lambda pq3=pq3, gsb=gsb, qg_t=qg_t, g=g, gk=gk, bo=bo: op("dve", lambda e: e.tensor_tensor(
                                out=qg_t[:].rearrange("p (h d) -> p h d", d=HD), in0=pq3,
                                in1=gsb[:].unsqueeze(1).to_broadcast([128, NH, HD]), op=ALU.mult),
                                reads=[("bank", bo + g), gk], writes=[("qg_t", g)]))
                            q3 = qg_t[:].rearrange("p (h d) -> p h d", d=HD)
                            cosb = cos_sb[:, i, :].unsqueeze(1).to_broadcast([128, NH, 32])
                            sinb = sin_sb[:, i, :].unsqueeze(1).to_broadcast([128, NH, 32])
                            x1v, x2v = q3[:, :, 0:32], q3[:, :, 32:64]
                            for ri, (xa, tb, tk) in enumerate(((x1v, cosb, "cos"), (x2v, sinb, "sin"), (x2v, cosb, "cos"), (x1v, sinb, "sin"))):
                                steps.append(lambda ri=ri, xa=xa, tb=tb, tk=tk, rt=rt, g=g, reng=reng: op(reng, lambda e: e.tensor_tensor(
                                    out=rt[ri][:], in0=xa, in1=tb, op=ALU.mult),
                                    reads=[("qg_t", g), tk], writes=[("rt", g, ri)]))
                            steps.append(lambda rt=rt, ro=ro, g=g, reng=reng: op(reng, lambda e: e.tensor_tensor(
                                out=ro[:, :, 0:32], in0=rt[0][:], in1=rt[1][:], op=ALU.subtract),
                                reads=[("rt", g, 0), ("rt", g, 1)], writes=[("ro1", g)]))
                            steps.append(lambda rt=rt, ro=ro, g=g, reng=reng: op(reng, lambda e: e.tensor_tensor(
                                out=ro[:, :, 32:64], in0=rt[2][:], in1=rt[3][:], op=ALU.add),
                                reads=[("rt", g, 2), ("rt", g, 3)], writes=[("ro2", g)]))
                            steps.append(lambda ro=ro, g=g, reng=reng: op(reng, lambda e: e.tensor_tensor(
                                out=qn[g][:].rearrange("p (h d) -> p h d", d=HD), in0=ro[:],
                                in1=ssq[:, g, :].unsqueeze(2).to_broadcast([128, NH, HD]), op=ALU.mult),
                                reads=[("ro1", g), ("ro2", g), ("ssq", g)], writes=[("qn", g)]))
                            chains.append(steps)
                        for sa, sb2 in zip(chains[0], chains[1]):
                            sa()
                            sb2()
                        for g in range(2):
                            for pr in range(4):
                                op("pe", lambda e, g=g, pr=pr: e.transpose(
                                    out=bankb[1][:, pr * 128:(pr + 1) * 128], in_=qn[g][:, pr * 128:(pr + 1) * 128],
                                    identity=ident_bf[:]),
                                   reads=[("qn", g)], writes=["bb1"])
                            if g == 0:
                                op("act", lambda e, i=i: e.copy(
                                    out=qT[:, :, i * 128:(i + 1) * 128],
                                    in_=bankb[1][:, 0:512].rearrange("p (k t) -> p k t", t=128)),
                                   reads=["bb1"], writes=[("qkT", g, i)])
                            else:
                                kv = kTp[:].rearrange("p (pr two) t -> p pr two t", two=2)
                                for hh in range(2):
                                    op("act", lambda e, i=i, hh=hh, kv=kv: e.copy(
                                        out=kv[64 * hh:64 * hh + 64, :, hh, i * 128:(i + 1) * 128],
                                        in_=bankb[1][64 * hh:64 * hh + 64, 0:512].rearrange("p (k t) -> p k t", t=128)),
                                       reads=["bb1"], writes=[("qkT", g, i, hh)])
                        op("act", lambda e, i=i: e.copy(
                            out=v1[:, i, :, 0:HD], in_=banks[bo + 2][:].rearrange("p (h d) -> p h d", d=HD)),
                           reads=[("bank", bo + 2)], writes=[("v1", i)])

                    def convab(c):
                        hs = c % 2
                        HTC = [("hT", hs, j) for j in range(4)]
                        for ct in range(4):
                            for ab in range(2):
                                col = 1536 + ab * 512 + ct * 128
                                for kt in range(8):
                                    op("pe", lambda e, ab=ab, kt=kt, hs=hs, col=col: e.matmul(
                                        banks[3 + ab][:, :], lhsT=win_sb[:, kt, col:col + 128], rhs=hT[hs][:, kt, :],
                                        start=(kt == 0), stop=(kt == 7)),
                                       reads=HTC + WAB[ab], writes=[("bank", 3 + ab)])
                            op("act", lambda e: e.activation(out=sg_sb[:], in_=banks[4][:], func=AF.Sigmoid),
                               reads=[("bank", 4)], writes=["sg"])
                            op("dve", lambda e, ct=ct, c=c: e.tensor_tensor(
                                out=u_sb[:, ct, 15 + c * 512: 15 + (c + 1) * 512], in0=banks[3][:], in1=sg_sb[:], op=ALU.mult),
                               reads=[("bank", 3), "sg"], writes=[("u", ct, c)])

                    front(0)
                    front(1)
                    front2(0)
                    for i in range(NT):
                        if i + 2 < NT:
                            front(i + 2)
                        if i + 1 < NT:
                            front2(i + 1)
                        chain(i)
                        if i % 4 == 3:
                            convab(i // 4)
                    A.emit(st, pool)

                st = ExitStack()
                with st:
                    def sb(name, shape, dt):
                        return st.enter_context(nc.sbuf_tensor(_u(name), list(shape), dt))

                    def ps(name, shape, dt):
                        return st.enter_context(nc.psum_tensor(_u(name), list(shape), dt))
                    cw_sb = sb("cw_sb", [128, 4, KC], F32)
                    cb_sb = sb("cb_sb", [128, 4], F32)
                    lg_sb = sb("lg_sb", [128, 4], F32)
                    lb_sb = sb("lb_sb", [128, 4], F32)
                    cT = sb("cT", [128, 4, S], F32)
                    dg = sb("dg", [128, 4, KC, 128], BF16)
                    sqt = [sb(f"sqt{i}", [128, 512], F32) for i in range(2)]
                    mean_sb = sb("mean_sb", [128, 512], F32)
                    lrs_sb = sb("lrs_sb", [128, 512], F32)
                    xc_sb = [sb(f"xc_sb{i}", [128, 512], F32) for i in range(2)]
                    banks = [ps(f"bank{i}", [128, 512], F32) for i in range(2)]
                    cbanks = [ps(f"cbank{i}", [128, 512], F32) for i in range(4)]
                    A = Sched(nc, f"P2a_{b}")
                    op, dma = A.op, A.dma
                    dma("sp", lambda e: e.dma_start(out=cw_sb[:], in_=cw), "c0", writes=["cw"])
                    dma("sp", lambda e: e.dma_start(out=cb_sb[:], in_=cb), "c1", writes=["cb"])
                    dma("sp", lambda e: e.dma_start(out=lg_sb[:], in_=lg), "c2", writes=["lg"])
                    dma("sp", lambda e: e.dma_start(out=lb_sb[:], in_=lb), "c3", writes=["lb"])
                    for ct in range(4):
                        op("dve", lambda e, ct=ct: e.tensor_tensor(
                            out=dg[:, ct, :, :], in0=ident_f[:].unsqueeze(1).to_broadcast([128, KC, 128]),
                            in1=cw_sb[:, ct, :].unsqueeze(2).to_broadcast([128, KC, 128]), op=ALU.mult),
                           reads=["cw"], writes=[("dg", ct)])
                    n_sq = 0
                    n_xc = 0

                    def ln_chunk(c):
                        nonlocal n_sq, n_xc
                        CTK = [("cT", ct, c) for ct in range(4)]
                        cs = slice(c * 512, (c + 1) * 512)
                        for ct in range(4):
                            op("pe", lambda e, ct=ct, cs=cs: e.matmul(banks[0][:, :], lhsT=ones_f[:], rhs=cT[:, ct, cs],
                                                                     start=(ct == 0), stop=(ct == 3)),
                               reads=CTK, writes=[("bank", 0)])
                        for ct in range(4):
                            sq = n_sq % 2
                            n_sq += 1
                            op("act", lambda e, ct=ct, cs=cs, sq=sq: e.activation(out=sqt[sq][:], in_=cT[:, ct, cs], func=AF.Square),
                               reads=CTK, writes=[("sqt", sq)])
                            op("pe", lambda e, ct=ct, sq=sq: e.matmul(banks[1][:, :], lhsT=ones_f[:], rhs=sqt[sq][:],
                                                                      start=(ct == 0), stop=(ct == 3)),
                               reads=[("sqt", sq)], writes=[("bank", 1)])
                        op("dve", lambda e: e.tensor_scalar(out=mean_sb[:], in0=banks[0][:], scalar1=1.0 / CW, scalar2=None, op0=ALU.mult),
                           reads=[("bank", 0)], writes=["mean"])
                        op("dve", lambda e: e.tensor_tensor(out=lrs_sb[:], in0=mean_sb[:], in1=mean_sb[:], op=ALU.mult),
                           reads=["mean"], writes=["lrs"])
                        op("dve", lambda e: e.scalar_tensor_tensor(out=lrs_sb[:], in0=banks[1][:], scalar=1.0 / CW, in1=lrs_sb[:],
                                                                   op0=ALU.mult, op1=ALU.subtract),
                           reads=[("bank", 1), "lrs"], writes=["lrs"])
                        op("dve", lambda e: e.tensor_scalar(out=lrs_sb[:], in0=lrs_sb[:], scalar1=EPS, scalar2=None, op0=ALU.add),
                           reads=["lrs"], writes=["lrs"])
                        op("act", lambda e: e.sqrt(out=lrs_sb[:], in_=lrs_sb[:]), reads=["lrs"], writes=["lrs"])
                        op("dve", lambda e: e.reciprocal(out=lrs_sb[:], in_=lrs_sb[:]), reads=["lrs"], writes=["lrs"])
                        for ct in range(4):
                            xs = n_xc % 2
                            n_xc += 1
                            op("dve", lambda e, ct=ct, cs=cs, xs=xs: e.tensor_tensor(out=xc_sb[xs][:], in0=cT[:, ct, cs], in1=mean_sb[:], op=ALU.subtract),
                               reads=CTK + ["mean"], writes=[("xc", xs)])
                            op("dve", lambda e, xs=xs: e.tensor_tensor(out=xc_sb[xs][:], in0=xc_sb[xs][:], in1=lrs_sb[:], op=ALU.mult),
                               reads=[("xc", xs), "lrs"], writes=[("xc", xs)])
                            op("act", lambda e, ct=ct, cs=cs, xs=xs: e.activation(
                                out=convT[:, ct, cs], in_=xc_sb[xs][:], func=AF.Silu, bias=lb_sb[:, ct:ct + 1], scale=lg_sb[:, ct:ct + 1]),
                               reads=[("xc", xs), "lg", "lb"], writes=[("convT", ct, c)])

                    ncb = 0
                    for _ in range(NWARM):
                        op("pe", lambda e: e.matmul(cbanks[0][:, :], lhsT=ident_bf[:], rhs=u_sb[:, 0, 0:512],
                                                    start=True, stop=True),
                           reads=[], writes=[("cbank", 0)])
                    for c in range(4):
                        for ct in range(4):
                            cbk = ncb % 4
                            ncb += 1
                            for k in range(KC):
                                op("pe", lambda e, cbk=cbk, ct=ct, k=k, c=c: e.matmul(
                                    cbanks[cbk][:, :], lhsT=dg[:, ct, k, :], rhs=u_sb[:, ct, c * 512 + k: c * 512 + k + 512],
                                    start=(k == 0), stop=(k == KC - 1)),
                                   reads=[("dg", ct)], writes=[("cbank", cbk)])
                            op("act", lambda e, cbk=cbk, ct=ct, c=c: e.activation(
                                out=cT[:, ct, c * 512:(c + 1) * 512], in_=cbanks[cbk][:, :], func=AF.Identity,
                                bias=cb_sb[:, ct:ct + 1], scale=1.0),
                               reads=[("cbank", cbk), "cb"], writes=[("cT", ct, c)])
                        if c > 0:
                            ln_chunk(c - 1)
                    ln_chunk(3)
                    A.emit(st, pool)

                st = ExitStack()
                with st:
                    def sb(name, shape, dt):
                        return st.enter_context(nc.sbuf_tensor(_u(name), list(shape), dt))

                    def ps(name, shape, dt):
                        return st.enter_context(nc.psum_tensor(_u(name), list(shape), dt))
                    woa_sb = sb("woa_sb", [128, NH, D], BF16)
                    woc_sb = sb("woc_sb", [128, 4, D], BF16)
                    wr_sb = sb("wr_sb", [128, 8, NE], BF16)
                    g2_sb = sb("g2_sb", [128, D], F32)
                    mask_sb = sb("mask_sb", [128, MU], BF16)
                    xt = [sb(f"xt{i}", [128, D], F32) for i in range(2)]
                    junk = sb("junk", [128, D], BF16)
                    st_ss = sb("st_ss", [128, 4], F32)
                    pt = [sb(f"pt{i}", [128, 512], BF16) for i in range(12)]
                    rden_bf = sb("rden_bf", [128, 512], BF16)
                    ones_bf = sb("ones_bf", [128, 64], BF16)
                    bc_sb = sb("bc_sb", [64, 512], F32)
                    attnT = [sb(f"attnT{i}", [128, NH, 512], BF16) for i in range(2)]
                    x1t = [sb(f"x1t{i}", [128, D], F32) for i in range(2)]
                    h2t = [sb(f"h2t{i}", [128, D], BF16) for i in range(2)]
                    h2T = sb("h2T", [128, 8, 128], BF16)
                    rsm = sb("rsm", [128, 8], F32)
                    re_sb = sb("re_sb", [128, NE], F32)
                    banks = [ps(f"bank{i}", [128, 512], F32) for i in range(8)]

                    A = Sched(nc, f"P2b_{b}")
                    op, dma = A.op, A.dma
                    dma("sp", lambda e: e.dma_start(out=g2_sb[:], in_=g2b), "c0", writes=["g2"])
                    dma("sp", lambda e: e.dma_start(out=mask_sb[:], in_=c_mask), "c1", writes=["mask"])
                    op("pool", lambda e: e.memset(ones_bf[:], 1.0), writes=["ones_bf"])
                    op("pool", lambda e: e.memset(woa_sb[64:128, :, :], 0.0), writes=["woa_z"])
                    for a_ in range(2):
                        op("pool", lambda e, a_=a_: e.memset(attnT[a_][64:128, :, :], 0.0), writes=[("attnT_z", a_)])
                    woa_v = w_out[0:512, :].rearrange("(h p) c -> p h c", p=64)
                    woc_v = w_out[512:1024, :].rearrange("(kt p) c -> p kt c", p=128)
                    for c0 in range(0, D, 512):
                        dma("pool", lambda e, c0=c0: e.dma_start(out=woa_sb[0:64, :, c0:c0 + 512], in_=woa_v[:, :, c0:c0 + 512]),
                            f"cw{c0 // 512}", writes=[("woa", c0)])
                        dma("pool", lambda e, c0=c0: e.dma_start(out=woc_sb[:, :, c0:c0 + 512], in_=woc_v[:, :, c0:c0 + 512]),
                            f"cw{2 + c0 // 512}", writes=[("woc", c0)])
                    dma("pool", lambda e: e.dma_start(out=wr_sb[:], in_=w_router.rearrange("(kt p) c -> p kt c", p=128)),
                        "cw4", writes=["wr"])
                    blks = []
                    for c in range(4):
                        klo, khi = max(0, 4 * c - 8), min(NT - 1, 4 * c + 3 + 8)
                        for h in range(NH):
                            for kb in range(klo, khi + 1):
                                blks.append((c, h, kb, kb == klo, kb == khi))
                    LA = 6
                    DEFER = 4
                    pending = []
                    defq = []
                    NSB = 5
                    sbanks = banks[0:5]
                    obanks = banks[5:7]
                    bbf = banks[1][:].bitcast(BF16)

                    def emit_S(n):
                        c, h, kb, first, last = blks[n]
                        hp, pair = 64 * (h % 2), h // 2
                        sb_ = n % NSB
                        pslot = n % 12
                        if h == 0 and first:
                            for _ in range(NWARM):
                                op("pe", lambda e, sb_=sb_: e.matmul(sbanks[sb_][:, :], lhsT=ident_bf[:], rhs=mask_sb[:, 0:512],
                                                                    start=True, stop=True),
                                   reads=["mask"], writes=[("sbank", sb_)])
                        op("pe", lambda e, sb_=sb_, hp=hp, pair=pair, kb=kb, c=c: e.matmul(
                            sbanks[sb_][:, :], lhsT=kTp[:, 2 * pair + hp // 64, kb * 128:(kb + 1) * 128],
                            rhs=qT[:, pair, c * 512:(c + 1) * 512], start=True, stop=True),
                           reads=[], writes=[("sbank", sb_)])
                        op("act", lambda e, sb_=sb_, pslot=pslot: e.activation(
                            out=pt[pslot][:], in_=sbanks[sb_][:], func=AF.Exp, scale=HD ** -0.5),
                           reads=[("sbank", sb_)], writes=[("pt", pslot)])
                        moff = 128 * (4 * c - kb) + MOFF
                        meng = "pool" if n % 4 == 3 else "dve"
                        op(meng, lambda e, pslot=pslot, moff=moff: e.tensor_tensor(
                            out=pt[pslot][:], in0=pt[pslot][:], in1=mask_sb[:, moff:moff + 512], op=ALU.mult),
                           reads=[("pt", pslot), "mask"], writes=[("pt", pslot)])

                    def emit_PV(n):
                        c, h, kb, first, last = blks[n]
                        ob = h % 2
                        pslot = n % 12
                        aslot = c % 2
                        op("pe", lambda e, ob=ob, kb=kb, h=h, pslot=pslot, first=first, last=last: e.matmul(
                            obanks[ob][:, :], lhsT=v1f[:, (kb * NH + h) * 65:(kb * NH + h) * 65 + 128], rhs=pt[pslot][:],
                            start=first, stop=last),
                           reads=[("pt", pslot)], writes=[("obank", ob)])
                        if not last:
                            return
                        def _rec(e, ob=ob):
                            with nc.allow_low_precision("bf16 operand of the denominator broadcast matmul"):
                                return e.reciprocal(out=rden_bf[64:65, :], in_=obanks[ob][64:65, :])
                        op("dve", _rec, reads=[("obank", ob)], writes=["rden"])
                        pending.append((n + DEFER, c, h, ob, aslot))

                    def fin_b(c, h, ob, aslot):
                        op("pe", lambda e: e.matmul(banks[7][0:64, :], lhsT=ones_bf[64:65, 0:64], rhs=rden_bf[64:65, :],
                                                    start=True, stop=True),
                           reads=["rden"], writes=[("bank", 7)])
                        op("act", lambda e: e.copy(out=bc_sb[:], in_=banks[7][0:64, :]), reads=[("bank", 7)], writes=["bc"])
                        op("dve", lambda e, ob=ob, aslot=aslot, h=h: e.tensor_tensor(
                            out=attnT[aslot][0:64, h, :], in0=obanks[ob][0:64, :], in1=bc_sb[:], op=ALU.mult),
                           reads=[("obank", ob), "bc"], writes=[("attnT", aslot, h)])
                        if h == NH - 1:
                            outproj(c)

                    def outproj(c):
                        aslot = c % 2
                        def px(j):
                            i = 4 * c + j
                            xs = i % 2
                            rows = slice(r0 + i * 128, r0 + (i + 1) * 128)
                            dma("sp", lambda e, xs=xs, rows=rows: e.dma_start(out=xt[xs][:], in_=x[rows, :]),
                                f"x{xs}", writes=[("xt", xs)])
                            for hf in range(2):
                                cols = slice(hf * 512, (hf + 1) * 512)
                                yb = 0
                                for h in range(NH):
                                    op("pe", lambda e, yb=yb, h=h, j=j, aslot=aslot, cols=cols: e.matmul(
                                        banks[yb][:, :], lhsT=attnT[aslot][:, h, j * 128:(j + 1) * 128], rhs=woa_sb[:, h, cols],
                                        start=(h == 0), stop=False),
                                       reads=[("attnT", aslot, h), ("woa", hf * 512), "woa_z", ("attnT_z", aslot)], writes=[("sbank", yb)])
                                for ct in range(4):
                                    op("pe", lambda e, yb=yb, ct=ct, i=i, cols=cols: e.matmul(
                                        banks[yb][:, :], lhsT=convT[:, ct, i * 128:(i + 1) * 128], rhs=woc_sb[:, ct, cols],
                                        start=False, stop=(ct == 3)),
                                       reads=[("woc", hf * 512)], writes=[("sbank", yb)])
                                op("dve", lambda e, yb=yb, xs=xs, cols=cols: e.tensor_tensor(
                                    out=x1t[xs][:, cols], in0=banks[yb][:, :], in1=xt[xs][:, cols], op=ALU.add),
                                   reads=[("sbank", yb), ("xt", xs)], writes=[("x1t", xs, hf)])
                            X1 = [("x1t", xs, 0), ("x1t", xs, 1)]
                            dma("sp", lambda e, xs=xs, rows=rows: e.dma_start(out=y[rows, :], in_=x1t[xs][:]),
                                f"y{xs}", reads=X1, writes=[("ydram", i)])
                            op("act", lambda e, xs=xs: e.activation(out=junk[:], in_=x1t[xs][:], func=AF.Square,
                                                                    accum_out=st_ss[:, 1:2]),
                               reads=X1, writes=["junk", "ss1"])
                            op("dve", lambda e: e.tensor_scalar(out=st_ss[:, 2:3], in0=st_ss[:, 1:2], scalar1=1.0 / D, scalar2=EPS,
                                                                op0=ALU.mult, op1=ALU.add), reads=["ss1"], writes=["rs2"])
                            op("act", lambda e: e.activation(out=st_ss[:, 2:3], in_=st_ss[:, 2:3], func=AF.Ln),
                               reads=["rs2"], writes=["rs2"])
                            op("act", lambda e: e.activation(out=st_ss[:, 2:3], in_=st_ss[:, 2:3], func=AF.Exp, scale=-0.5),
                               reads=["rs2"], writes=["rs2"])
                            op("dve", lambda e, xs=xs: e.scalar_tensor_tensor(
                                out=h2t[xs][:], in0=x1t[xs][:], scalar=st_ss[:, 2:3], in1=g2_sb[:],
                                op0=ALU.mult, op1=ALU.mult),
                               reads=X1 + ["rs2", "g2"], writes=[("h2t", xs)])
                            dma("sp", lambda e, xs=xs, rows=rows: e.dma_start(out=h2s[rows, :], in_=h2t[xs][:]),
                                f"h{xs}", reads=[("h2t", xs)], writes=[("h2dram", i)])

                        def py_(j):
                            i = 4 * c + j
                            xs = i % 2
                            for kt in range(8):
                                op("pe", lambda e, xs=xs, kt=kt: e.transpose(
                                    out=bbf[:, kt * 128:(kt + 1) * 128], in_=h2t[xs][:, kt * 128:(kt + 1) * 128],
                                    identity=ident_bf[:]),
                                   reads=[("h2t", xs)], writes=[("sbank", 1)])
                            op("act", lambda e: e.copy(out=h2T[:], in_=bbf.rearrange("p (k t) -> p k t", t=128)),
                               reads=[("sbank", 1)], writes=["h2T"])
                            for kt in range(8):
                                op("pe", lambda e, kt=kt: e.matmul(banks[7][:, 0:NE], lhsT=h2T[:, kt, :], rhs=wr_sb[:, kt, :],
                                                                   start=(kt == 0), stop=(kt == 7)),
                                   reads=["h2T", "wr"], writes=[("bank", 7)])
                            op("dve", lambda e: e.tensor_reduce(out=rsm[:, 0:1], in_=banks[7][:, 0:NE], axis=AX.X, op=ALU.max),
                               reads=[("bank", 7)], writes=["rsm0"])
                            op("dve", lambda e: e.tensor_scalar(out=rsm[:, 1:2], in0=rsm[:, 0:1], scalar1=-1.0, scalar2=None, op0=ALU.mult),
                               reads=["rsm0"], writes=["rsm1"])
                            op("act", lambda e: e.activation(out=re_sb[:], in_=banks[7][:, 0:NE], func=AF.Exp,
                                                             bias=rsm[:, 1:2], scale=1.0, accum_out=rsm[:, 2:3]),
                               reads=[("bank", 7), "rsm1"], writes=["re", "rsm2"])
                            op("dve", lambda e: e.reciprocal(out=rsm[:, 3:4], in_=rsm[:, 2:3]), reads=["rsm2"], writes=["rsm3"])
                            op("dve", lambda e, i=i, b=b: e.tensor_scalar(
                                out=aff_all[:, i, b, :], in0=re_sb[:], scalar1=rsm[:, 3:4], scalar2=None, op0=ALU.mult),
                               reads=["re", "rsm3"], writes=[("aff", i, b)])

                        for j in range(4):
                            defq.append(lambda j=j: px(j))
                            if j > 0:
                                defq.append(lambda j=j: py_(j - 1))
                        defq.append(lambda: py_(3))

                    G = 2
                    for n0 in range(0, len(blks) + LA + G, G):
                        for n in range(n0, n0 + G):
                            if n < len(blks):
                                emit_S(n)
                        for n in range(n0, n0 + G):
                            if 0 <= n - LA < len(blks):
                                emit_PV(n - LA)
                        while pending and pending[0][0] <= n0 + G - 1 - LA:
                            _, c_, h_, ob_, as_ = pending.pop(0)
                            fin_b(c_, h_, ob_, as_)
                        if defq and (n0 // G) % 4 == 3:
                            defq.pop(0)()
                    while pending:
                        _, c_, h_, ob_, as_ = pending.pop(0)
                        fin_b(c_, h_, ob_, as_)
                    while defq:
                        defq.pop(0)()
                    A.emit(st, pool)

        st = ExitStack()
        with st:
            def sb(name, shape, dt):
                return st.enter_context(nc.sbuf_tensor(_u(name), list(shape), dt))

            def ps(name, shape, dt):
                return st.enter_context(nc.psum_tensor(_u(name), list(shape), dt))
            offs_sb = sb("offs_sb", [NP, 1], F32)
            affT = sb("affT", [NP, S], F32)
            affW = sb("affW", [NP, S], F32)
            gate_sb = sb("gate_sb", [NP, CAP], F32)
            idxu = sb("idxu", [NP, CAP], U32)
            idxf = sb("idxf", [NP, CAP], F32)
            banks = [ps(f"bank{i}", [128, 512], F32) for i in range(3)]
            A = Sched(nc, "R")
            op, dma = A.op, A.dma
            dma("sp", lambda e: e.dma_start(out=offs_sb[:], in_=c_offs), "c0", writes=["offs"])
            for i in range(NT):
                op("pe", lambda e, i=i: e.transpose(
                    out=banks[0][0:NP, 0:128], in_=aff_all[:, i, :, :].rearrange("p b e -> p (b e)"),
                    identity=ident_f[:]),
                   reads=[], writes=[("bank", 0)])
                op("dve", lambda e, i=i: e.tensor_copy(out=affT[:, i * 128:(i + 1) * 128], in_=banks[0][0:NP, 0:128]),
                   reads=[("bank", 0)], writes=["affT"])
            for r in range(CAP // 8):
                src = affT if r == 0 else affW
                skey = "affT" if r == 0 else "affW"
                sl = slice(8 * r, 8 * r + 8)
                op("dve", lambda e, src=src, sl=sl: e.max(out=gate_sb[:, sl], in_=src[:]), reads=[skey], writes=[("gate", r)])
                op("dve", lambda e, src=src, sl=sl: e.max_index(out=idxu[:, sl], in_max=gate_sb[:, sl], in_values=src[:]),
                   reads=[skey, ("gate", r)], writes=[("idxu", r)])
                op("dve", lambda e, src=src, sl=sl: e.match_replace(out=affW[:], in_to_replace=gate_sb[:, sl],
                                                                   in_values=src[:], imm_value=-1.0),
                   reads=[skey, ("gate", r)], writes=["affW"])
            op("dve", lambda e: e.tensor_copy(out=idxf[:], in_=idxu[:]),
               reads=[("idxu", r) for r in range(CAP // 8)], writes=["idxf"])
            op("dve", lambda e: e.tensor_scalar(out=idxf[:], in0=idxf[:], scalar1=offs_sb[:, 0:1], scalar2=None, op0=ALU.add),
               reads=["idxf", "offs"], writes=["idxf"])
            for hf in range(2):
                op("pe", lambda e, hf=hf: e.transpose(out=banks[1][:, 0:NP], in_=idxf[:, hf * 128:(hf + 1) * 128],
                                                      identity=ident_f[0:NP, 0:NP]),
                   reads=["idxf"], writes=[("bank", 1)])
                op("dve", lambda e, hf=hf: e.tensor_copy(out=idxT[:, hf, :], in_=banks[1][:, 0:NP]),
                   reads=[("bank", 1)], writes=[("idxT", hf)])
                op("pe", lambda e, hf=hf: e.transpose(out=banks[2][:, 0:NP], in_=gate_sb[:, hf * 128:(hf + 1) * 128],
                                                      identity=ident_f[0:NP, 0:NP]),
                   reads=[("gate", r) for r in range(CAP // 8)], writes=[("bank", 2)])
                op("dve", lambda e, hf=hf: e.tensor_copy(out=gateT[:, hf, :], in_=banks[2][:, 0:NP]),
                   reads=[("bank", 2)], writes=[("gateT", hf)])
            A.emit(st, pool)

        stB = ExitStack()
        with stB:
            def sb(name, shape, dt):
                return stB.enter_context(nc.sbuf_tensor(_u(name), list(shape), dt))

            def ps(name, shape, dt):
                return stB.enter_context(nc.psum_tensor(_u(name), list(shape), dt))

            TE = NB * CAP
            NM = TE // 128
            NW = min(512, TE)
            NTH = TE // NW
            FG = [(f0, min(4, NFT - f0)) for f0 in range(0, NFT, 4)]
            w1s = [sb(f"w1s{i}", [128, 8, 512], BF16) for i in range(3)]
            w3s = [sb(f"w3s{i}", [128, 8, 512], BF16) for i in range(3)]
            w2s = sb("w2s", [128, NFT, D], BF16)
            act_sb = sb("act_sb", [128, NFT, TE], BF16)
            hTs = [sb(f"hTs{i}", [128, 8, TE], BF16) for i in range(2)]
            grow = [sb(f"grow{i}", [128, D], BF16) for i in range(4)]
            sgb = [sb(f"sgb{i}", [128, NW], F32) for i in range(2)]
            ye = [sb(f"ye{i}", [128, D], F32) for i in range(2)]
            pg = [ps(f"pg{i}", [128, 512], F32) for i in range(4)]
            py = [ps(f"py{i}", [128, 512], F32) for i in range(2)]
            ptr = [ps(f"ptr{i}", [128, 1024], BF16) for i in range(2)]

            B = Sched(nc, "B")
            op, dma = B.op, B.dma
            gcount = 0

            def gather_expert(ex):
                nonlocal gcount
                hs = ex % 2
                for m in range(NM):
                    b, hf = m // 2, m % 2
                    gs = gcount % 4
                    ts = gcount % 2
                    gcount += 1
                    col = b * NE + ex
                    dma("pool", lambda e, gs=gs, hf=hf, col=col: e.indirect_dma_start(
                        out=grow[gs][:], out_offset=None, in_=h2s,
                        in_offset=bass.IndirectOffsetOnAxis(ap=idxT[:, hf, col:col + 1], axis=0)),
                        f"g{gs}", writes=[("grow", gs)])
                    for kt in range(8):
                        op("pe", lambda e, gs=gs, ts=ts, kt=kt: e.transpose(
                            out=ptr[ts][:, kt * 128:(kt + 1) * 128], in_=grow[gs][:, kt * 128:(kt + 1) * 128],
                            identity=ident_bf[:]),
                           reads=[("grow", gs)], writes=[("ptr", ts)])
                    op("act", lambda e, hs=hs, ts=ts, m=m: e.copy(
                        out=hTs[hs][:, :, m * 128:(m + 1) * 128], in_=ptr[ts][:].rearrange("p (k t) -> p k t", t=128)),
                       reads=[("ptr", ts)], writes=[("hTs", hs, m)])

            loads = [(ex, gi) for ex in range(NE) for gi in range(len(FG))]

            def issue_w13(li):
                ex, gi = loads[li]
                f0, nf = FG[gi]
                ws = li % 3
                c0, c1 = f0 * 128, (f0 + nf) * 128
                dma("pool", lambda e, ws=ws, ex=ex, c0=c0, c1=c1: e.dma_start(
                    out=w1s[ws][:, :, 0:c1 - c0], in_=w1[ex].rearrange("(kt p) f -> p kt f", p=128)[:, :, c0:c1]),
                    f"w1_{ws}", writes=[("w1s", ws)])
                dma("pool", lambda e, ws=ws, ex=ex, c0=c0, c1=c1: e.dma_start(
                    out=w3s[ws][:, :, 0:c1 - c0], in_=w3[ex].rearrange("(kt p) f -> p kt f", p=128)[:, :, c0:c1]),
                    f"w3_{ws}", writes=[("w3s", ws)])

            def issue_w2(ex):
                w2v = w2[ex].rearrange("(ft p) d -> p ft d", p=128)
                for f0 in range(0, NFT, 2):
                    dma("pool", lambda e, f0=f0, w2v=w2v: e.dma_start(out=w2s[:, f0:f0 + 2, :], in_=w2v[:, f0:f0 + 2, :]),
                        f"w2_{f0}", writes=[("w2s", f0)])

            gather_expert(0)
            issue_w13(0)
            issue_w13(1)
            li = 0
            gsl = 0
            ysl = 0
            for ex in range(NE):
                hs = ex % 2
                issue_w2(ex)
                for gi, (f0, nf) in enumerate(FG):
                    ws = li % 3
                    if li + 2 < len(loads):
                        issue_w13(li + 2)
                    li += 1
                    for fi in range(nf):
                        f = f0 + fi
                        for th in range(NTH):
                            ts_ = slice(th * NW, (th + 1) * NW)
                            HK = [("hTs", hs, m) for m in range(th * NW // 128, (th + 1) * NW // 128)]
                            g1b_, g3b_ = pg[2 * (gsl % 2)], pg[2 * (gsl % 2) + 1]
                            k1, k3 = ("pg", 2 * (gsl % 2)), ("pg", 2 * (gsl % 2) + 1)
                            sgs = gsl % 2
                            gsl += 1
                            for kt in range(8):
                                op("pe", lambda e, g1b_=g1b_, ws=ws, kt=kt, fi=fi, hs=hs, ts_=ts_: e.matmul(
                                    g1b_[:, 0:NW], lhsT=w1s[ws][:, kt, fi * 128:(fi + 1) * 128], rhs=hTs[hs][:, kt, ts_],
                                    start=(kt == 0), stop=(kt == 7)),
                                   reads=HK + [("w1s", ws)], writes=[k1])
                            for kt in range(8):
                                op("pe", lambda e, g3b_=g3b_, ws=ws, kt=kt, fi=fi, hs=hs, ts_=ts_: e.matmul(
                                    g3b_[:, 0:NW], lhsT=w3s[ws][:, kt, fi * 128:(fi + 1) * 128], rhs=hTs[hs][:, kt, ts_],
                                    start=(kt == 0), stop=(kt == 7)),
                                   reads=HK + [("w3s", ws)], writes=[k3])
                            op("act", lambda e, sgs=sgs, g1b_=g1b_: e.activation(out=sgb[sgs][:], in_=g1b_[:, 0:NW], func=AF.Silu),
                               reads=[k1], writes=[("sgb", sgs)])
                            op("dve", lambda e, sgs=sgs, g3b_=g3b_, f=f, ts_=ts_: e.tensor_tensor(
                                out=act_sb[:, f, ts_], in0=g3b_[:, 0:NW], in1=sgb[sgs][:], op=ALU.mult),
                               reads=[k3, ("sgb", sgs)], writes=[("act", f, th)])
                if ex + 1 < NE:
                    gather_expert(ex + 1)
                W2K = [("w2s", f0) for f0 in range(0, NFT, 2)]
                for m in range(NM):
                    b, hf = m // 2, m % 2
                    col = b * NE + ex
                    th = (m * 128) // NW
                    ys = ysl % 2
                    ysl += 1
                    for dh in range(2):
                        for f in range(NFT):
                            op("pe", lambda e, dh=dh, f=f, m=m: e.matmul(
                                py[dh][:, :], lhsT=act_sb[:, f, m * 128:(m + 1) * 128], rhs=w2s[:, f, dh * 512:(dh + 1) * 512],
                                start=(f == 0), stop=(f == NFT - 1)),
                               reads=[("act", f, th), ("w2s", f - f % 2)], writes=[("py", dh)])
                        if dh == 0:
                            op("act", lambda e, ys=ys, dh=dh, hf=hf, col=col: e.activation(
                                out=ye[ys][:, dh * 512:(dh + 1) * 512], in_=py[dh][:, :], func=AF.Copy,
                                scale=gateT[:, hf, col:col + 1]),
                               reads=[("py", dh)], writes=[("ye", ys, dh)])
                        else:
                            op("dve", lambda e, ys=ys, dh=dh, hf=hf, col=col: e.tensor_scalar(
                                out=ye[ys][:, dh * 512:(dh + 1) * 512], in0=py[dh][:, :],
                                scalar1=gateT[:, hf, col:col + 1], scalar2=None, op0=ALU.mult),
                               reads=[("py", dh)], writes=[("ye", ys, dh)])
                    par = ex % 2
                    dma("pool", lambda e, ys=ys, hf=hf, col=col: e.indirect_dma_start(
                        out=y, out_offset=bass.IndirectOffsetOnAxis(ap=idxT[:, hf, col:col + 1], axis=0),
                        in_=ye[ys][:], in_offset=None, compute_op=ALU.add),
                        f"sc{ys}", reads=[("ye", ys, 0), ("ye", ys, 1)] + [("ysc", 1 - par, mm) for mm in range(NM)],
                        writes=[("ysc", par, m)])
            B.emit(stB, pool)
    return nc


_NC_CACHE = {}


def _get_nc(NB):
    if NB not in _NC_CACHE:
        _NC_CACHE[NB] = build_nc(NB)
    return _NC_CACHE[NB]


def make_in_maps(inputs, NB, n_cores):
    f = lambda a: np.ascontiguousarray(np.asarray(a, dtype=np.float32))
    x = f(inputs["x"]).reshape(-1, D)
    consts = host_consts(NB)
    shared = dict(
        g1b=np.ascontiguousarray(np.broadcast_to(f(inputs["norm1_g"])[None, :], (128, D))),
        g2b=np.ascontiguousarray(np.broadcast_to(f(inputs["norm2_g"])[None, :], (128, D))),
        w_in=f(inputs["w_in"]),
        qgb=np.ascontiguousarray(np.broadcast_to(f(inputs["q_norm_g"])[None, :], (128, HD))),
        kgb=np.ascontiguousarray(np.broadcast_to(f(inputs["k_norm_g"])[None, :], (128, HD))),
        cw=np.ascontiguousarray(f(inputs["conv_dw_w"]).reshape(KC, 4, 128).transpose(2, 1, 0)),
        cb=np.ascontiguousarray(f(inputs["conv_dw_b"]).reshape(4, 128).T),
        lg=np.ascontiguousarray(f(inputs["conv_ln_g"]).reshape(4, 128).T),
        lb=np.ascontiguousarray(f(inputs["conv_ln_b"]).reshape(4, 128).T),
        w_out=f(inputs["w_out"]),
        w_router=f(inputs["w_router"]),
        w1=f(inputs["w1"]), w3=f(inputs["w3"]), w2=f(inputs["w2"]),
        **consts,
    )
    maps = []
    for c in range(n_cores):
        m = dict(shared)
        m["x"] = x[c * NB * S:(c + 1) * NB * S]
        maps.append(m)
    return maps


def kernel(**inputs):
    Bt = np.asarray(inputs["x"]).shape[0]
    NB = Bt // N_CORES
    nc = _get_nc(NB)
    in_maps = make_in_maps(inputs, NB, N_CORES)
    res = run_bass_kernel_spmd(nc, in_maps, core_ids=list(range(N_CORES)))
    out = np.concatenate([np.asarray(r["y"]) for r in res.results], axis=0)
    return out.reshape(Bt, S, D).astype(np.float32, copy=False)
```

```python
import numpy as np
from contextlib import ExitStack
import ml_dtypes
import concourse.bass as bass
import concourse.mybir as mybir
from concourse.bass_utils import run_bass_kernel_spmd

F32 = mybir.dt.float32
BF16 = mybir.dt.bfloat16
I32 = mybir.dt.int32
U32 = mybir.dt.uint32
AF = mybir.ActivationFunctionType
ALU = mybir.AluOpType
AX = mybir.AxisListType

ENGS = ("pe", "act", "dve", "pool", "sp")

D = 1024
S = 2048
NT = 16
HD = 64
NH = 8
CW = 512
KC = 31
NE = 16
CAP = 256
DFF = 2816
NFT = DFF // 128
EPS = 1e-6
MOFF = 1920
MU = 3968
N_CORES = 8
NWARM = 0


_UID = [0]
LIMIT = [None]


def _u(name):
    _UID[0] += 1
    return f"{name}_{_UID[0]}"


class SemPool:
    def __init__(self, nc, stack):
        self.nc = nc
        self.stack = stack
        self.esem = {e: stack.enter_context(nc.semaphore(f"s_{e}")) for e in ENGS}
        self.ecount = {e: 0 for e in ENGS}
        self.dsem = {}
        self.dcount = {}
        self.nblocks = 0

    def stream(self, s):
        if s not in self.dsem:
            self.dsem[s] = self.stack.enter_context(self.nc.semaphore(f"d_{s}"))
            self.dcount[s] = 0
        return self.dsem[s]


class Sched:
    def __init__(self, nc, name):
        self.nc = nc
        self.name = name
        self.ops = {e: [] for e in ENGS}
        self.last_w = {}
        self.readers = {}
        self.streams = {}
        self.stream_order = []

    def _deps(self, reads, writes):
        deps = []
        for r in reads:
            t = self.last_w.get(r)
            if t is not None:
                deps.append(t)
        for w in writes:
            t = self.last_w.get(w)
            if t is not None:
                deps.append(t)
            deps.extend(self.readers.get(w, ()))
        return deps

    def _commit(self, tok, reads, writes):
        for r in reads:
            self.readers.setdefault(r, []).append(tok)
        for w in writes:
            self.last_w[w] = tok
            self.readers[w] = []

    def op(self, eng, fn, reads=(), writes=()):
        deps = self._deps(reads, writes)
        tok = ("e", eng, len(self.ops[eng]))
        self.ops[eng].append(dict(fn=fn, deps=deps, ms=False, dma=None))
        self._commit(tok, reads, writes)
        return tok

    def dma(self, q, fn, stream, reads=(), writes=()):
        deps = self._deps(reads, writes)
        if stream not in self.streams:
            self.streams[stream] = 0
            self.stream_order.append(stream)
        self.streams[stream] += 1
        tok = ("d", stream, self.streams[stream])
        self.ops[q].append(dict(fn=fn, deps=deps, ms=False, dma=stream))
        self._commit(tok, reads, writes)
        return tok

    def wait_all(self, eng, keys):
        self.op(eng, None, reads=list(keys), writes=list(keys))

    def emit(self, stack, pool):
        nc = self.nc
        pool.nblocks += 1
        if LIMIT[0] is not None and pool.nblocks > LIMIT[0]:
            return
        for e in ENGS:
            for rec in self.ops[e]:
                for d in rec["deps"]:
                    if d[0] == "e" and not (d[1] == "pe" and e == "pe"):
                        self.ops[d[1]][d[2]]["ms"] = True
        msval = {}
        nms = {}
        for e in ENGS:
            c = pool.ecount[e]
            for i, rec in enumerate(self.ops[e]):
                if rec["ms"]:
                    c += 1
                    msval[(e, i)] = c
            nms[e] = c
        esem = pool.esem
        dsem = {s: pool.stream(s) for s in self.stream_order}
        dbase = {s: pool.dcount[s] for s in self.stream_order}
        block = stack.enter_context(nc.Block())
        engobj = {"pe": block.tensor, "act": block.scalar, "dve": block.vector,
                  "pool": block.gpsimd, "sp": block.sync}

        def run(e):
            def body(eng):
                waited = {}
                for rec in self.ops[e]:
                    need = {}
                    for d in rec["deps"]:
                        if d[0] == "e":
                            if d[1] == "pe" and e == "pe":
                                continue
                            key = ("e", d[1])
                            val = msval[(d[1], d[2])]
                        else:
                            key = ("d", d[1])
                            val = 16 * (dbase[d[1]] + d[2])
                        if val > need.get(key, 0):
                            need[key] = val
                    for key, val in need.items():
                        if waited.get(key, 0) >= val:
                            continue
                        waited[key] = val
                        sem = esem[key[1]] if key[0] == "e" else dsem[key[1]]
                        eng.wait_ge(sem, val)
                    if rec["fn"] is None:
                        if rec["ms"]:
                            eng.nop().then_inc(esem[e], 1)
                        continue
                    ins = rec["fn"](eng)
                    if rec["dma"] is not None:
                        ins.then_inc(dsem[rec["dma"]], 16)
                    elif rec["ms"]:
                        ins.then_inc(esem[e], 1)
                if e == "sp":
                    for s in self.stream_order:
                        eng.wait_ge(dsem[s], 16 * (dbase[s] + self.streams[s]))
            return body

        for e in ENGS:
            engobj[e](run(e))
        for e in ENGS:
            pool.ecount[e] = nms[e]
        for s in self.stream_order:
            pool.dcount[s] += self.streams[s]


def _count(d):
    ad = abs(d)
    c = 0
    if ad <= 64:
        c += 1
    if d % 4 == 0 and ad <= 256:
        c += 1
    if d % 16 == 0 and ad <= 1024:
        c += 1
    return c


def host_consts(NB):
    half = HD // 2
    inv_freq = (10000.0 ** (-np.arange(half, dtype=np.float32) / half)).astype(np.float32)
    pos = np.arange(S, dtype=np.float32)
    ang = pos[:, None] * inv_freq[None, :]
    cos = np.cos(ang).astype(np.float32).reshape(NT, 128, half).transpose(1, 0, 2)
    sin = np.sin(ang).astype(np.float32).reshape(NT, 128, half).transpose(1, 0, 2)
    cvals = np.array([_count(d) for d in range(-(MU + 128), MU + 128)], dtype=np.float32)
    u = np.arange(MU)[None, :]
    p = np.arange(128)[:, None]
    dd = u - p - MOFF
    maskT = cvals[dd + (MU + 128)].astype(ml_dtypes.bfloat16)
    offs = (np.arange(NB * NE) // NE * S).astype(np.float32).reshape(NB * NE, 1)
    return dict(
        c_cos=np.ascontiguousarray(cos), c_sin=np.ascontiguousarray(sin),
        c_mask=np.ascontiguousarray(maskT),
        c_ident=np.eye(128, dtype=np.float32),
        c_offs=offs,
    )


def build_nc(NB):
    nc = bass.Bass("TRN2", target_bir_lowering=False)
    NTOK = NB * S
    NP = NB * NE

    def din(name, shape, dt=F32):
        return nc.dram_tensor(name, list(shape), dt, kind="ExternalInput").ap()

    x = din("x", [NTOK, D])
    g1b = din("g1b", [128, D])
    g2b = din("g2b", [128, D])
    w_in = din("w_in", [D, 2560])
    qgb = din("qgb", [128, HD])
    kgb = din("kgb", [128, HD])
    cw = din("cw", [128, 4, KC])
    cb = din("cb", [128, 4])
    lg = din("lg", [128, 4])
    lb = din("lb", [128, 4])
    w_out = din("w_out", [D, D])
    w_router = din("w_router", [D, NE])
    w1 = din("w1", [NE, D, DFF])
    w3 = din("w3", [NE, D, DFF])
    w2 = din("w2", [NE, DFF, D])
    c_cos = din("c_cos", [128, NT, 32])
    c_sin = din("c_sin", [128, NT, 32])
    c_mask = din("c_mask", [128, MU], BF16)
    c_ident = din("c_ident", [128, 128])
    c_offs = din("c_offs", [NP, 1])
    y = nc.dram_tensor("y", [NTOK, D], F32, kind="ExternalOutput").ap()
    h2s = nc.dram_tensor("h2s", [NTOK, D], BF16, kind="Internal").ap()

    outer = ExitStack()
    with outer:
        pool = SemPool(nc, outer)

        def sbo(name, shape, dt):
            return outer.enter_context(nc.sbuf_tensor(_u(name), list(shape), dt))

        idxT = sbo("idxT", [128, 2, NP], I32)
        gateT = sbo("gateT", [128, 2, NP], F32)
        ident_bf = sbo("ident_bf", [128, 128], BF16)
        ident_f = sbo("ident_f", [128, 128], F32)
        ones_f = sbo("ones_f", [128, 128], F32)
        aff_all = sbo("aff_all", [128, NT, NB, NE], F32)

        def rstd_chain(op, ss_ap, out_ap, n, keyin, keyout):
            op("dve", lambda e: e.tensor_scalar(out=out_ap, in0=ss_ap, scalar1=1.0 / n, scalar2=EPS,
                                                op0=ALU.mult, op1=ALU.add), reads=[keyin], writes=[keyout])
            op("act", lambda e: e.sqrt(out=out_ap, in_=out_ap), reads=[keyout], writes=[keyout])
            op("dve", lambda e: e.reciprocal(out=out_ap, in_=out_ap), reads=[keyout], writes=[keyout])

        stSeq = ExitStack()
        with stSeq:
            def sbq(name, shape, dt):
                return stSeq.enter_context(nc.sbuf_tensor(_u(name), list(shape), dt))
            qT = sbq("qT", [128, 4, S], BF16)
            kTp = sbq("kTp", [128, NH, S], BF16)
            v1f = sbq("v1f", [128, NT * NH * 65 + 64], BF16)
            v1 = v1f[:, 0:NT * NH * 65].rearrange("p (t h d) -> p t h d", h=NH, d=65)
            u_sb = sbq("u_sb", [128, 4, S + 30], BF16)
            convT = sbq("convT", [128, 4, S], BF16)

            st = ExitStack()
            with st:
                I = Sched(nc, "I")
                I.dma("sp", lambda e: e.dma_start(out=ident_f[:], in_=c_ident), "c0", writes=["ident_f"])
                I.op("dve", lambda e: e.tensor_copy(out=ident_bf[:], in_=ident_f[:]), reads=["ident_f"], writes=["ident_bf"])
                I.op("pool", lambda e: e.memset(ones_f[:], 1.0), writes=["ones_f"])
                I.op("pool", lambda e: e.memset(v1f[:], 1.0), writes=["v1_ones"])
                I.op("pool", lambda e: e.memset(kTp[:], 0.0), writes=["kTp_zero"])
                I.op("pool", lambda e: e.memset(u_sb[:], 0.0), writes=["u_zero"])
                I.emit(st, pool)

            for b in range(NB):
                r0 = b * S
                st = ExitStack()
                with st:
                    def sb(name, shape, dt):
                        return st.enter_context(nc.sbuf_tensor(_u(name), list(shape), dt))

                    def ps(name, shape, dt):
                        return st.enter_context(nc.psum_tensor(_u(name), list(shape), dt))
                    win_sb = sb("win_sb", [128, 8, 2560], BF16)
                    g1_sb = sb("g1_sb", [128, D], F32)
                    qg_sb = sb("qg_sb", [128, HD], F32)
                    kg_sb = sb("kg_sb", [128, HD], F32)
                    cos_sb = sb("cos_sb", [128, NT, 32], F32)
                    sin_sb = sb("sin_sb", [128, NT, 32], F32)
                    xt = [sb(f"xt{i}", [128, D], F32) for i in range(2)]
                    junk = sb("junk", [128, D], BF16)
                    hb = [sb(f"hb{i}", [128, D], BF16) for i in range(2)]
                    hT = [sb(f"hT{i}", [128, 8, 512], BF16) for i in range(2)]
                    st_ss = sb("st_ss", [128, 4], F32)
                    rstd_all = sb("rstd_all", [128, NT], F32)
                    sqt2 = [sb(f"sqt{g}", [128, 512], F32) for g in range(2)]
                    ssq = sb("ssq", [128, 2, NH], F32)
                    qg_t2 = [sb(f"qg_t{g}", [128, 512], F32) for g in range(2)]
                    rt2 = [[sb(f"rt{g}_{i}", [128, NH, 32], F32) for i in range(4)] for g in range(2)]
                    ro2 = [sb(f"ro{g}", [128, NH, HD], F32) for g in range(2)]
                    qn = [sb(f"qn{i}", [128, 512], BF16) for i in range(2)]
                    sg_sb = sb("sg_sb", [128, 512], F32)
                    banks = [ps(f"bank{i}", [128, 512], F32) for i in range(6)]
                    bankb = [ps(f"bankb{i}", [128, 1024], BF16) for i in range(2)]

                    A = Sched(nc, f"P1_{b}")
                    op, dma = A.op, A.dma
                    dma("sp", lambda e: e.dma_start(out=g1_sb[:], in_=g1b), "c0", writes=["g1"])
                    dma("sp", lambda e: e.dma_start(out=qg_sb[:], in_=qgb), "c1", writes=["qg"])
                    dma("sp", lambda e: e.dma_start(out=kg_sb[:], in_=kgb), "c2", writes=["kg"])
                    dma("sp", lambda e: e.dma_start(out=cos_sb[:], in_=c_cos), "c3", writes=["cos"])
                    dma("sp", lambda e: e.dma_start(out=sin_sb[:], in_=c_sin), "c4", writes=["sin"])
                    win_v = w_in.rearrange("(kt p) c -> p kt c", p=128)
                    for c0 in range(0, 2560, 512):
                        dma("pool", lambda e, c0=c0: e.dma_start(out=win_sb[:, :, c0:c0 + 512], in_=win_v[:, :, c0:c0 + 512]),
                            f"cw{c0 // 512}", writes=[("win", c0)])
                    WQKV = [[("win", g * 512)] for g in range(3)]
                    WAB = [[("win", 1536 + ab * 512)] for ab in range(2)]

                    def front(i):
                        c, j = i // 4, i % 4
                        hs, xs, bo = c % 2, i % 2, 3 * (i % 2)
                        rows = slice(r0 + i * 128, r0 + (i + 1) * 128)
                        dma("sp", lambda e, xs=xs, rows=rows: e.dma_start(out=xt[xs][:], in_=x[rows, :]),
                            f"x{xs}", writes=[("xt", xs)])
                        op("act", lambda e, xs=xs: e.activation(out=junk[:], in_=xt[xs][:], func=AF.Square,
                                                                accum_out=st_ss[:, 0:1]),
                           reads=[("xt", xs)], writes=["junk", "ss0"])
                        rstd_chain(op, st_ss[:, 0:1], rstd_all[:, i:i + 1], D, "ss0", ("rstd", i))
                        op("dve", lambda e, xs=xs, i=i: e.scalar_tensor_tensor(
                            out=hb[xs][:], in0=xt[xs][:], scalar=rstd_all[:, i:i + 1], in1=g1_sb[:],
                            op0=ALU.mult, op1=ALU.mult),
                           reads=[("xt", xs), ("rstd", i), "g1"], writes=[("hb", xs)])

                    def front2(i):
                        c, j = i // 4, i % 4
                        hs, xs, bo = c % 2, i % 2, 3 * (i % 2)
                        for kt in range(8):
                            op("pe", lambda e, xs=xs, kt=kt: e.transpose(
                                out=bankb[0][:, kt * 128:(kt + 1) * 128], in_=hb[xs][:, kt * 128:(kt + 1) * 128],
                                identity=ident_bf[:]),
                               reads=[("hb", xs)], writes=["bb0"])
                        op("act", lambda e, hs=hs, j=j: e.copy(
                            out=hT[hs][:, :, j * 128:(j + 1) * 128],
                            in_=bankb[0][:].rearrange("p (k t) -> p k t", t=128)),
                           reads=["bb0"], writes=[("hT", hs, j)])
                        if j == 0:
                            for _ in range(NWARM):
                                op("pe", lambda e: e.matmul(banks[bo][:, :], lhsT=ident_bf[:], rhs=win_sb[:, 0, 0:512],
                                                            start=True, stop=True),
                                   reads=[("win", 0)], writes=[("bank", bo)])
                        for g in range(3):
                            for kt in range(8):
                                op("pe", lambda e, g=g, kt=kt, hs=hs, j=j: e.matmul(
                                    banks[bo + g][:, :], lhsT=hT[hs][:, kt, j * 128:(j + 1) * 128],
                                    rhs=win_sb[:, kt, g * 512:(g + 1) * 512], start=(kt == 0), stop=(kt == 7)),
                                   reads=[("hT", hs, j)] + WQKV[g], writes=[("bank", bo + g)])

                    def chain(i):
                        bo = 3 * (i % 2)
                        chains = []
                        for g, gsb, gk in ((0, qg_sb, "qg"), (1, kg_sb, "kg")):
                            steps = []
                            pq = banks[bo + g]
                            pq3 = pq[:].rearrange("p (h d) -> p h d", d=HD)
                            sqt, qg_t, rt, ro = sqt2[g], qg_t2[g], rt2[g], ro2[g]
                            reng = "dve" if g == 0 else "pool"
                            K_ = lambda s, g=g: (s, g)
                            steps.append(lambda pq=pq, sqt=sqt, g=g, bo=bo: op("act", lambda e: e.activation(out=sqt[:], in_=pq[:], func=AF.Square),
                                         reads=[("bank", bo + g)], writes=[("sqt", g)]))
                            steps.append(lambda sqt=sqt, g=g: op("dve", lambda e: e.tensor_reduce(
                                out=ssq[:, g, :], in_=sqt[:].rearrange("p (h d) -> p h d", d=HD), axis=AX.X, op=ALU.add),
                                reads=[("sqt", g)], writes=[("ssq", g)]))
                            steps.append(lambda g=g: op("dve", lambda e: e.tensor_scalar(
                                out=ssq[:, g, :], in0=ssq[:, g, :], scalar1=1.0 / HD, scalar2=EPS, op0=ALU.mult, op1=ALU.add),
                                reads=[("ssq", g)], writes=[("ssq", g)]))
                            steps.append(lambda g=g: op("act", lambda e: e.sqrt(out=ssq[:, g, :], in_=ssq[:, g, :]),
                                                        reads=[("ssq", g)], writes=[("ssq", g)]))
                            steps.append(lambda g=g: op("dve", lambda e: e.reciprocal(out=ssq[:, g, :], in_=ssq[:, g, :]),
                                                        reads=[("ssq", g)], writes=[("ssq", g)]))
                            steps.append(lambda pq3=pq3, gsb=gsb, qg_t=qg_t, g=g, gk=gk, bo=bo: op("dve", lambda e: e.tensor_tensor(
                                out=qg_t[:].rearrange("p (h d) -> p h d", d=HD), in0=pq3,
                                in1=gsb[:].unsqueeze(1).to_broadcast([128, NH, HD]), op=ALU.mult),
                                reads=[("bank", bo + g), gk], writes=[("qg_t", g)]))
                            q3 = qg_t[:].rearrange("p (h d) -> p h d", d=HD)
                            cosb = cos_sb[:, i, :].unsqueeze(1).to_broadcast([128, NH, 32])
                            sinb = sin_sb[:, i, :].unsqueeze(1).to_broadcast([128, NH, 32])
                            x1v, x2v = q3[:, :, 0:32], q3[:, :, 32:64]
                            for ri, (xa, tb, tk) in enumerate(((x1v, cosb, "cos"), (x2v, sinb, "sin"), (x2v, cosb, "cos"), (x1v, sinb, "sin"))):
                                steps.append(lambda ri=ri, xa=xa, tb=tb, tk=tk, rt=rt, g=g, reng=reng: op(reng, lambda e: e.tensor_tensor(
                                    out=rt[ri][:], in0=xa, in1=tb, op=ALU.mult),
                                    reads=[("qg_t", g), tk], writes=[("rt", g, ri)]))
                            steps.append(lambda rt=rt, ro=ro, g=g, reng=reng: op(reng, lambda e: e.tensor_tensor(
                                out=ro[:, :, 0:32], in0=rt[0][:], in1=rt[1][:], op=ALU.subtract),
                                reads=[("rt", g, 0), ("rt", g, 1)], writes=[("ro1", g)]))
                            steps.append(lambda rt=rt, ro=ro, g=g, reng=reng: op(reng, lambda e: e.tensor_tensor(
                                out=ro[:, :, 32:64], in0=rt[2][:], in1=rt[3][:], op=ALU.add),
                                reads=[("rt", g, 2), ("rt", g, 3)], writes=[("ro2", g)]))
                            steps.append(lambda ro=ro, g=g, reng=reng: op(reng, lambda e: e.tensor_tensor(
                                out=qn[g][:].rearrange("p (h d) -> p h d", d=HD), in0=ro[:],
                                in1=ssq[:, g, :].unsqueeze(2).to_broadcast([128, NH, HD]), op=ALU.mult),
                                reads=[("ro1", g), ("ro2", g), ("ssq", g)], writes=[("qn", g)]))
                            chains.append(steps)
                        for sa, sb2 in zip(chains[0], chains[1]):
                            sa()
                            sb2()
                        for g in range(2):
                            for pr in range(4):
                                op("pe", lambda e, g=g, pr=pr: e.transpose(
                                    out=bankb[1][:, pr * 128:(pr + 1) * 128], in_=qn[g][:, pr * 128:(pr + 1) * 128],
                                    identity=ident_bf[:]),
                                   reads=[("qn", g)], writes=["bb1"])
                            if g == 0:
                                op("act", lambda e, i=i: e.copy(
                                    out=qT[:, :, i * 128:(i + 1) * 128],
                                    in_=bankb[1][:, 0:512].rearrange("p (k t) -> p k t", t=128)),
                                   reads=["bb1"], writes=[("qkT", g, i)])
                            else:
                                kv = kTp[:].rearrange("p (pr two) t -> p pr two t", two=2)
                                for hh in range(2):
                                    op("act", lambda e, i=i, hh=hh, kv=kv: e.copy(
                                        out=kv[64 * hh:64 * hh + 64, :, hh, i * 128:(i + 1) * 128],
                                        in_=bankb[1][64 * hh:64 * hh + 64, 0:512].rearrange("p (k t) -> p k t", t=128)),
                                       reads=["bb1"], writes=[("qkT", g, i, hh)])
                        op("act", lambda e, i=i: e.copy(
                            out=v1[:, i, :, 0:HD], in_=banks[bo + 2][:].rearrange("p (h d) -> p h d", d=HD)),
                           reads=[("bank", bo + 2)], writes=[("v1", i)])

                    def convab(c):
                        hs = c % 2
                        HTC = [("hT", hs, j) for j in range(4)]
                        for ct in range(4):
                            for ab in range(2):
                                col = 1536 + ab * 512 + ct * 128
                                for kt in range(8):
                                    op("pe", lambda e, ab=ab, kt=kt, hs=hs, col=col: e.matmul(
                                        banks[3 + ab][:, :], lhsT=win_sb[:, kt, col:col + 128], rhs=hT[hs][:, kt, :],
                                        start=(kt == 0), stop=(kt == 7)),
                                       reads=HTC + WAB[ab], writes=[("bank", 3 + ab)])
                            op("act", lambda e: e.activation(out=sg_sb[:], in_=banks[4][:], func=AF.Sigmoid),
                               reads=[("bank", 4)], writes=["sg"])
                            op("dve", lambda e, ct=ct, c=c: e.tensor_tensor(
                                out=u_sb[:, ct, 15 + c * 512: 15 + (c + 1) * 512], in0=banks[3][:], in1=sg_sb[:], op=ALU.mult),
                               reads=[("bank", 3), "sg"], writes=[("u", ct, c)])

                    front(0)
                    front(1)
                    front2(0)
                    for i in range(NT):
                        if i + 2 < NT:
                            front(i + 2)
                        if i + 1 < NT:
                            front2(i + 1)
                        chain(i)
                        if i % 4 == 3:
                            convab(i // 4)
                    A.emit(st, pool)

                st = ExitStack()
                with st:
                    def sb(name, shape, dt):
                        return st.enter_context(nc.sbuf_tensor(_u(name), list(shape), dt))

                    def ps(name, shape, dt):
                        return st.enter_context(nc.psum_tensor(_u(name), list(shape), dt))
                    cw_sb = sb("cw_sb", [128, 4, KC], F32)
                    cb_sb = sb("cb_sb", [128, 4], F32)
                    lg_sb = sb("lg_sb", [128, 4], F32)
                    lb_sb = sb("lb_sb", [128, 4], F32)
                    cT = sb("cT", [128, 4, S], F32)
                    dg = sb("dg", [128, 4, KC, 128], BF16)
                    sqt = [sb(f"sqt{i}", [128, 512], F32) for i in range(2)]
                    mean_sb = sb("mean_sb", [128, 512], F32)
                    lrs_sb = sb("lrs_sb", [128, 512], F32)
                    xc_sb = [sb(f"xc_sb{i}", [128, 512], F32) for i in range(2)]
                    banks = [ps(f"bank{i}", [128, 512], F32) for i in range(2)]
                    cbanks = [ps(f"cbank{i}", [128, 512], F32) for i in range(4)]
                    A = Sched(nc, f"P2a_{b}")
                    op, dma = A.op, A.dma
                    dma("sp", lambda e: e.dma_start(out=cw_sb[:], in_=cw), "c0", writes=["cw"])
                    dma("sp", lambda e: e.dma_start(out=cb_sb[:], in_=cb), "c1", writes=["cb"])
                    dma("sp", lambda e: e.dma_start(out=lg_sb[:], in_=lg), "c2", writes=["lg"])
                    dma("sp", lambda e: e.dma_start(out=lb_sb[:], in_=lb), "c3", writes=["lb"])
                    for ct in range(4):
                        op("dve", lambda e, ct=ct: e.tensor_tensor(
                            out=dg[:, ct, :, :], in0=ident_f[:].unsqueeze(1).to_broadcast([128, KC, 128]),
                            in1=cw_sb[:, ct, :].unsqueeze(2).to_broadcast([128, KC, 128]), op=ALU.mult),
                           reads=["cw"], writes=[("dg", ct)])
                    n_sq = 0
                    n_xc = 0

                    def ln_chunk(c):
                        nonlocal n_sq, n_xc
                        CTK = [("cT", ct, c) for ct in range(4)]
                        cs = slice(c * 512, (c + 1) * 512)
                        for ct in range(4):
                            op("pe", lambda e, ct=ct, cs=cs: e.matmul(banks[0][:, :], lhsT=ones_f[:], rhs=cT[:, ct, cs],
                                                                     start=(ct == 0), stop=(ct == 3)),
                               reads=CTK, writes=[("bank", 0)])
                        for ct in range(4):
                            sq = n_sq % 2
                            n_sq += 1
                            op("act", lambda e, ct=ct, cs=cs, sq=sq: e.activation(out=sqt[sq][:], in_=cT[:, ct, cs], func=AF.Square),
                               reads=CTK, writes=[("sqt", sq)])
                            op("pe", lambda e, ct=ct, sq=sq: e.matmul(banks[1][:, :], lhsT=ones_f[:], rhs=sqt[sq][:],
                                                                      start=(ct == 0), stop=(ct == 3)),
                               reads=[("sqt", sq)], writes=[("bank", 1)])
                        op("dve", lambda e: e.tensor_scalar(out=mean_sb[:], in0=banks[0][:], scalar1=1.0 / CW, scalar2=None, op0=ALU.mult),
                           reads=[("bank", 0)], writes=["mean"])
                        op("dve", lambda e: e.tensor_tensor(out=lrs_sb[:], in0=mean_sb[:], in1=mean_sb[:], op=ALU.mult),
                           reads=["mean"], writes=["lrs"])
                        op("dve", lambda e: e.scalar_tensor_tensor(out=lrs_sb[:], in0=banks[1][:], scalar=1.0 / CW, in1=lrs_sb[:],
                                                                   op0=ALU.mult, op1=ALU.subtract),
                           reads=[("bank", 1), "lrs"], writes=["lrs"])
                        op("dve", lambda e: e.tensor_scalar(out=lrs_sb[:], in0=lrs_sb[:], scalar1=EPS, scalar2=None, op0=ALU.add),
                           reads=["lrs"], writes=["lrs"])
                        op("act", lambda e: e.sqrt(out=lrs_sb[:], in_=lrs_sb[:]), reads=["lrs"], writes=["lrs"])
                        op("dve", lambda e: e.reciprocal(out=lrs_sb[:], in_=lrs_sb[:]), reads=["lrs"], writes=["lrs"])
                        for ct in range(4):
                            xs = n_xc % 2
                            n_xc += 1
                            op("dve", lambda e, ct=ct, cs=cs, xs=xs: e.tensor_tensor(out=xc_sb[xs][:], in0=cT[:, ct, cs], in1=mean_sb[:], op=ALU.subtract),
                               reads=CTK + ["mean"], writes=[("xc", xs)])
                            op("dve", lambda e, xs=xs: e.tensor_tensor(out=xc_sb[xs][:], in0=xc_sb[xs][:], in1=lrs_sb[:], op=ALU.mult),
                               reads=[("xc", xs), "lrs"], writes=[("xc", xs)])
                            op("act", lambda e, ct=ct, cs=cs, xs=xs: e.activation(
                                out=convT[:, ct, cs], in_=xc_sb[xs][:], func=AF.Silu, bias=lb_sb[:, ct:ct + 1], scale=lg_sb[:, ct:ct + 1]),
                               reads=[("xc", xs), "lg", "lb"], writes=[("convT", ct, c)])

                    ncb = 0
                    for _ in range(NWARM):
                        op("pe", lambda e: e.matmul(cbanks[0][:, :], lhsT=ident_bf[:], rhs=u_sb[:, 0, 0:512],
                                                    start=True, stop=True),
                           reads=[], writes=[("cbank", 0)])
                    for c in range(4):
                        for ct in range(4):
                            cbk = ncb % 4
                            ncb += 1
                            for k in range(KC):
                                op("pe", lambda e, cbk=cbk, ct=ct, k=k, c=c: e.matmul(
                                    cbanks[cbk][:, :], lhsT=dg[:, ct, k, :], rhs=u_sb[:, ct, c * 512 + k: c * 512 + k + 512],
                                    start=(k == 0), stop=(k == KC - 1)),
                                   reads=[("dg", ct)], writes=[("cbank", cbk)])
                            op("act", lambda e, cbk=cbk, ct=ct, c=c: e.activation(
                                out=cT[:, ct, c * 512:(c + 1) * 512], in_=cbanks[cbk][:, :], func=AF.Identity,
                                bias=cb_sb[:, ct:ct + 1], scale=1.0),
                               reads=[("cbank", cbk), "cb"], writes=[("cT", ct, c)])
                        if c > 0:
                            ln_chunk(c - 1)
                    ln_chunk(3)
                    A.emit(st, pool)

                st = ExitStack()
                with st:
                    def sb(name, shape, dt):
                        return st.enter_context(nc.sbuf_tensor(_u(name), list(shape), dt))

                    def ps(name, shape, dt):
                        return st.enter_context(nc.psum_tensor(_u(name), list(shape), dt))
                    woa_sb = sb("woa_sb", [128, NH, D], BF16)
                    woc_sb = sb("woc_sb", [128, 4, D], BF16)
                    wr_sb = sb("wr_sb", [128, 8, NE], BF16)
                    g2_sb = sb("g2_sb", [128, D], F32)
                    mask_sb = sb("mask_sb", [128, MU], BF16)
                    xt = [sb(f"xt{i}", [128, D], F32) for i in range(2)]
                    junk = sb("junk", [128, D], BF16)
                    st_ss = sb("st_ss", [128, 4], F32)
                    pt = [sb(f"pt{i}", [128, 512], BF16) for i in range(12)]
                    rden_bf = sb("rden_bf", [128, 512], BF16)
                    rden_f = sb("rden_f", [128, 512], F32)
                    ones_bf = sb("ones_bf", [128, 64], BF16)
                    bc_sb = sb("bc_sb", [64, 512], F32)
                    attnT = [sb(f"attnT{i}", [128, NH, 512], BF16) for i in range(2)]
                    x1t = [sb(f"x1t{i}", [128, D], F32) for i in range(2)]
                    h2t = [sb(f"h2t{i}", [128, D], BF16) for i in range(2)]
                    h2T = sb("h2T", [128, 8, 128], BF16)
                    rsm = sb("rsm", [128, 8], F32)
                    re_sb = sb("re_sb", [128, NE], F32)
                    banks = [ps(f"bank{i}", [128, 512], F32) for i in range(8)]

                    A = Sched(nc, f"P2b_{b}")
                    op, dma = A.op, A.dma
                    dma("sp", lambda e: e.dma_start(out=g2_sb[:], in_=g2b), "c0", writes=["g2"])
                    dma("sp", lambda e: e.dma_start(out=mask_sb[:], in_=c_mask), "c1", writes=["mask"])
                    op("pool", lambda e: e.memset(ones_bf[:], 1.0), writes=["ones_bf"])
                    op("pool", lambda e: e.memset(woa_sb[64:128, :, :], 0.0), writes=["woa_z"])
                    for a_ in range(2):
                        op("pool", lambda e, a_=a_: e.memset(attnT[a_][64:128, :, :], 0.0), writes=[("attnT_z", a_)])
                    woa_v = w_out[0:512, :].rearrange("(h p) c -> p h c", p=64)
                    woc_v = w_out[512:1024, :].rearrange("(kt p) c -> p kt c", p=128)
                    for c0 in range(0, D, 512):
                        dma("pool", lambda e, c0=c0: e.dma_start(out=woa_sb[0:64, :, c0:c0 + 512], in_=woa_v[:, :, c0:c0 + 512]),
                            f"cw{c0 // 512}", writes=[("woa", c0)])
                        dma("pool", lambda e, c0=c0: e.dma_start(out=woc_sb[:, :, c0:c0 + 512], in_=woc_v[:, :, c0:c0 + 512]),
                            f"cw{2 + c0 // 512}", writes=[("woc", c0)])
                    dma("pool", lambda e: e.dma_start(out=wr_sb[:], in_=w_router.rearrange("(kt p) c -> p kt c", p=128)),
                        "cw4", writes=["wr"])
                    blks = []
                    for c in range(4):
                        klo, khi = max(0, 4 * c - 8), min(NT - 1, 4 * c + 3 + 8)
                        for h in range(NH):
                            for kb in range(klo, khi + 1):
                                blks.append((c, h, kb, kb == klo, kb == khi))
                    LA = 6
                    DEFER = 4
                    pending = []
                    defq = []
                    NSB = 5
                    sbanks = banks[0:5]
                    obanks = banks[5:7]
                    bbf = banks[1][:].bitcast(BF16)

                    def emit_S(n):
                        c, h, kb, first, last = blks[n]
                        hp, pair = 64 * (h % 2), h // 2
                        sb_ = n % NSB
                        pslot = n % 12
                        if h == 0 and first:
                            for _ in range(NWARM):
                                op("pe", lambda e, sb_=sb_: e.matmul(sbanks[sb_][:, :], lhsT=ident_bf[:], rhs=mask_sb[:, 0:512],
                                                                    start=True, stop=True),
                                   reads=["mask"], writes=[("sbank", sb_)])
                        op("pe", lambda e, sb_=sb_, hp=hp, pair=pair, kb=kb, c=c: e.matmul(
                            sbanks[sb_][:, :], lhsT=kTp[:, 2 * pair + hp // 64, kb * 128:(kb + 1) * 128],
                            rhs=qT[:, pair, c * 512:(c + 1) * 512], start=True, stop=True),
                           reads=[], writes=[("sbank", sb_)])
                        op("act", lambda e, sb_=sb_, pslot=pslot: e.activation(
                            out=pt[pslot][:], in_=sbanks[sb_][:], func=AF.Exp, scale=HD ** -0.5),
                           reads=[("sbank", sb_)], writes=[("pt", pslot)])
                        moff = 128 * (4 * c - kb) + MOFF
                        meng = "pool" if n % 4 == 3 else "dve"
                        op(meng, lambda e, pslot=pslot, moff=moff: e.tensor_tensor(
                            out=pt[pslot][:], in0=pt[pslot][:], in1=mask_sb[:, moff:moff + 512], op=ALU.mult),
                           reads=[("pt", pslot), "mask"], writes=[("pt", pslot)])

                    def emit_PV(n):
                        c, h, kb, first, last = blks[n]
                        ob = h % 2
                        pslot = n % 12
                        aslot = c % 2
                        op("pe", lambda e, ob=ob, kb=kb, h=h, pslot=pslot, first=first, last=last: e.matmul(
                            obanks[ob][:, :], lhsT=v1f[:, (kb * NH + h) * 65:(kb * NH + h) * 65 + 128], rhs=pt[pslot][:],
                            start=first, stop=last),
                           reads=[("pt", pslot)], writes=[("obank", ob)])
                        if not last:
                            return
                        op("act", lambda e, ob=ob: e.activation(out=rden_f[64:65, :], in_=obanks[ob][64:65, :], func=AF.Ln),
                           reads=[("obank", ob)], writes=["rden_f"])
                        op("act", lambda e: e.activation(out=rden_bf[64:65, :], in_=rden_f[64:65, :], func=AF.Exp, scale=-1.0),
                           reads=["rden_f"], writes=["rden"])
                        pending.append((n + DEFER, c, h, ob, aslot))

                    def fin_b(c, h, ob, aslot):
                        op("pe", lambda e: e.matmul(banks[7][0:64, :], lhsT=ones_bf[64:65, 0:64], rhs=rden_bf[64:65, :],
                                                    start=True, stop=True),
                           reads=["rden"], writes=[("bank", 7)])
                        op("act", lambda e: e.copy(out=bc_sb[:], in_=banks[7][0:64, :]), reads=[("bank", 7)], writes=["bc"])
                        op("dve", lambda e, ob=ob, aslot=aslot, h=h: e.tensor_tensor(
                            out=attnT[aslot][0:64, h, :], in0=obanks[ob][0:64, :], in1=bc_sb[:], op=ALU.mult),
                           reads=[("obank", ob), "bc"], writes=[("attnT", aslot, h)])
                        if h == NH - 1:
                            outproj(c)

                    def outproj(c):
                        aslot = c % 2
                        def px(j):
                            i = 4 * c + j
                            xs = i % 2
                            rows = slice(r0 + i * 128, r0 + (i + 1) * 128)
                            dma("sp", lambda e, xs=xs, rows=rows: e.dma_start(out=xt[xs][:], in_=x[rows, :]),
                                f"x{xs}", writes=[("xt", xs)])
                            for hf in range(2):
                                cols = slice(hf * 512, (hf + 1) * 512)
                                yb = 0
                                for h in range(NH):
                                    op("pe", lambda e, yb=yb, h=h, j=j, aslot=aslot, cols=cols: e.matmul(
                                        banks[yb][:, :], lhsT=attnT[aslot][:, h, j * 128:(j + 1) * 128], rhs=woa_sb[:, h, cols],
                                        start=(h == 0), stop=False),
                                       reads=[("attnT", aslot, h), ("woa", hf * 512), "woa_z", ("attnT_z", aslot)], writes=[("sbank", yb)])
                                for ct in range(4):
                                    op("pe", lambda e, yb=yb, ct=ct, i=i, cols=cols: e.matmul(
                                        banks[yb][:, :], lhsT=convT[:, ct, i * 128:(i + 1) * 128], rhs=woc_sb[:, ct, cols],
                                        start=False, stop=(ct == 3)),
                                       reads=[("woc", hf * 512)], writes=[("sbank", yb)])
                                op("dve", lambda e, yb=yb, xs=xs, cols=cols: e.tensor_tensor(
                                    out=x1t[xs][:, cols], in0=banks[yb][:, :], in1=xt[xs][:, cols], op=ALU.add),
                                   reads=[("sbank", yb), ("xt", xs)], writes=[("x1t", xs, hf)])
                            X1 = [("x1t", xs, 0), ("x1t", xs, 1)]
                            dma("sp", lambda e, xs=xs, rows=rows: e.dma_start(out=y[rows, :], in_=x1t[xs][:]),
                                f"y{xs}", reads=X1, writes=[("ydram", i)])
                            op("act", lambda e, xs=xs: e.activation(out=junk[:], in_=x1t[xs][:], func=AF.Square,
                                                                    accum_out=st_ss[:, 1:2]),
                               reads=X1, writes=["junk", "ss1"])
                            op("dve", lambda e: e.tensor_scalar(out=st_ss[:, 2:3], in0=st_ss[:, 1:2], scalar1=1.0 / D, scalar2=EPS,
                                                                op0=ALU.mult, op1=ALU.add), reads=["ss1"], writes=["rs2"])
                            op("act", lambda e: e.activation(out=st_ss[:, 2:3], in_=st_ss[:, 2:3], func=AF.Ln),
                               reads=["rs2"], writes=["rs2"])
                            op("act", lambda e: e.activation(out=st_ss[:, 2:3], in_=st_ss[:, 2:3], func=AF.Exp, scale=-0.5),
                               reads=["rs2"], writes=["rs2"])
                            op("dve", lambda e, xs=xs: e.scalar_tensor_tensor(
                                out=h2t[xs][:], in0=x1t[xs][:], scalar=st_ss[:, 2:3], in1=g2_sb[:],
                                op0=ALU.mult, op1=ALU.mult),
                               reads=X1 + ["rs2", "g2"], writes=[("h2t", xs)])
                            dma("sp", lambda e, xs=xs, rows=rows: e.dma_start(out=h2s[rows, :], in_=h2t[xs][:]),
                                f"h{xs}", reads=[("h2t", xs)], writes=[("h2dram", i)])

                        def py_(j):
                            i = 4 * c + j
                            xs = i % 2
                            for kt in range(8):
                                op("pe", lambda e, xs=xs, kt=kt: e.transpose(
                                    out=bbf[:, kt * 128:(kt + 1) * 128], in_=h2t[xs][:, kt * 128:(kt + 1) * 128],
                                    identity=ident_bf[:]),
                                   reads=[("h2t", xs)], writes=[("sbank", 1)])
                            op("act", lambda e: e.copy(out=h2T[:], in_=bbf.rearrange("p (k t) -> p k t", t=128)),
                               reads=[("sbank", 1)], writes=["h2T"])
                            for kt in range(8):
                                op("pe", lambda e, kt=kt: e.matmul(banks[7][:, 0:NE], lhsT=h2T[:, kt, :], rhs=wr_sb[:, kt, :],
                                                                   start=(kt == 0), stop=(kt == 7)),
                                   reads=["h2T", "wr"], writes=[("bank", 7)])
                            op("dve", lambda e: e.tensor_reduce(out=rsm[:, 0:1], in_=banks[7][:, 0:NE], axis=AX.X, op=ALU.max),
                               reads=[("bank", 7)], writes=["rsm0"])
                            op("dve", lambda e: e.tensor_scalar(out=rsm[:, 1:2], in0=rsm[:, 0:1], scalar1=-1.0, scalar2=None, op0=ALU.mult),
                               reads=["rsm0"], writes=["rsm1"])
                            op("act", lambda e: e.activation(out=re_sb[:], in_=banks[7][:, 0:NE], func=AF.Exp,
                                                             bias=rsm[:, 1:2], scale=1.0, accum_out=rsm[:, 2:3]),
                               reads=[("bank", 7), "rsm1"], writes=["re", "rsm2"])
                            op("dve", lambda e: e.reciprocal(out=rsm[:, 3:4], in_=rsm[:, 2:3]), reads=["rsm2"], writes=["rsm3"])
                            op("dve", lambda e, i=i, b=b: e.tensor_scalar(
                                out=aff_all[:, i, b, :], in0=re_sb[:], scalar1=rsm[:, 3:4], scalar2=None, op0=ALU.mult),
                               reads=["re", "rsm3"], writes=[("aff", i, b)])

                        for j in range(4):
                            defq.append(lambda j=j: px(j))
                            if j > 0:
                                defq.append(lambda j=j: py_(j - 1))
                        defq.append(lambda: py_(3))

                    G = 2
                    for n0 in range(0, len(blks) + LA + G, G):
                        for n in range(n0, n0 + G):
                            if n < len(blks):
                                emit_S(n)
                        for n in range(n0, n0 + G):
                            if 0 <= n - LA < len(blks):
                                emit_PV(n - LA)
                        while pending and pending[0][0] <= n0 + G - 1 - LA:
                            _, c_, h_, ob_, as_ = pending.pop(0)
                            fin_b(c_, h_, ob_, as_)
                        if defq and (n0 // G) % 4 == 3:
                            defq.pop(0)()
                    while pending:
                        _, c_, h_, ob_, as_ = pending.pop(0)
                        fin_b(c_, h_, ob_, as_)
                    while defq:
                        defq.pop(0)()
                    A.emit(st, pool)

        st = ExitStack()
        with st:
            def sb(name, shape, dt):
                return st.enter_context(nc.sbuf_tensor(_u(name), list(shape), dt))

            def ps(name, shape, dt):
                return st.enter_context(nc.psum_tensor(_u(name), list(shape), dt))
            offs_sb = sb("offs_sb", [NP, 1], F32)
            affT = sb("affT", [NP, S], F32)
            affW = sb("affW", [NP, S], F32)
            gate_sb = sb("gate_sb", [NP, CAP], F32)
            idxu = sb("idxu", [NP, CAP], U32)
            idxf = sb("idxf", [NP, CAP], F32)
            banks = [ps(f"bank{i}", [128, 512], F32) for i in range(3)]
            A = Sched(nc, "R")
            op, dma = A.op, A.dma
            dma("sp", lambda e: e.dma_start(out=offs_sb[:], in_=c_offs), "c0", writes=["offs"])
            for i in range(NT):
                op("pe", lambda e, i=i: e.transpose(
                    out=banks[0][0:NP, 0:128], in_=aff_all[:, i, :, :].rearrange("p b e -> p (b e)"),
                    identity=ident_f[:]),
                   reads=[], writes=[("bank", 0)])
                op("dve", lambda e, i=i: e.tensor_copy(out=affT[:, i * 128:(i + 1) * 128], in_=banks[0][0:NP, 0:128]),
                   reads=[("bank", 0)], writes=["affT"])
            for r in range(CAP // 8):
                src = affT if r == 0 else affW
                skey = "affT" if r == 0 else "affW"
                sl = slice(8 * r, 8 * r + 8)
                op("dve", lambda e, src=src, sl=sl: e.max(out=gate_sb[:, sl], in_=src[:]), reads=[skey], writes=[("gate", r)])
                op("dve", lambda e, src=src, sl=sl: e.max_index(out=idxu[:, sl], in_max=gate_sb[:, sl], in_values=src[:]),
                   reads=[skey, ("gate", r)], writes=[("idxu", r)])
                op("dve", lambda e, src=src, sl=sl: e.match_replace(out=affW[:], in_to_replace=gate_sb[:, sl],
                                                                   in_values=src[:], imm_value=-1.0),
                   reads=[skey, ("gate", r)], writes=["affW"])
            op("dve", lambda e: e.tensor_copy(out=idxf[:], in_=idxu[:]),
               reads=[("idxu", r) for r in range(CAP // 8)], writes=["idxf"])
            op("dve", lambda e: e.tensor_scalar(out=idxf[:], in0=idxf[:], scalar1=offs_sb[:, 0:1], scalar2=None, op0=ALU.add),
               reads=["idxf", "offs"], writes=["idxf"])
            for hf in range(2):
                op("pe", lambda e, hf=hf: e.transpose(out=banks[1][:, 0:NP], in_=idxf[:, hf * 128:(hf + 1) * 128],
                                                      identity=ident_f[0:NP, 0:NP]),
                   reads=["idxf"], writes=[("bank", 1)])
                op("dve", lambda e, hf=hf: e.tensor_copy(out=idxT[:, hf, :], in_=banks[1][:, 0:NP]),
                   reads=[("bank", 1)], writes=[("idxT", hf)])
                op("pe", lambda e, hf=hf: e.transpose(out=banks[2][:, 0:NP], in_=gate_sb[:, hf * 128:(hf + 1) * 128],
                                                      identity=ident_f[0:NP, 0:NP]),
                   reads=[("gate", r) for r in range(CAP // 8)], writes=[("bank", 2)])
                op("dve", lambda e, hf=hf: e.tensor_copy(out=gateT[:, hf, :], in_=banks[2][:, 0:NP]),
                   reads=[("bank", 2)], writes=[("gateT", hf)])
            A.emit(st, pool)

        stB = ExitStack()
        with stB:
            def sb(name, shape, dt):
                return stB.enter_context(nc.sbuf_tensor(_u(name), list(shape), dt))

            def ps(name, shape, dt):
                return stB.enter_context(nc.psum_tensor(_u(name), list(shape), dt))

            TE = NB * CAP
            NM = TE // 128
            NW = min(512, TE)
            NTH = TE // NW
            FG = [(f0, min(4, NFT - f0)) for f0 in range(0, NFT, 4)]
            w1s = [sb(f"w1s{i}", [128, 8, 512], BF16) for i in range(3)]
            w3s = [sb(f"w3s{i}", [128, 8, 512], BF16) for i in range(3)]
            w2s = sb("w2s", [128, NFT, D], BF16)
            act_sb = sb("act_sb", [128, NFT, TE], BF16)
            hTs = [sb(f"hTs{i}", [128, 8, TE], BF16) for i in range(2)]
            grow = [sb(f"grow{i}", [128, D], BF16) for i in range(4)]
            sgb = [sb(f"sgb{i}", [128, NW], F32) for i in range(2)]
            ye = [sb(f"ye{i}", [128, D], F32) for i in range(2)]
            pg = [ps(f"pg{i}", [128, 512], F32) for i in range(4)]
            py = [ps(f"py{i}", [128, 512], F32) for i in range(2)]
            ptr = [ps(f"ptr{i}", [128, 1024], BF16) for i in range(2)]

            B = Sched(nc, "B")
            op, dma = B.op, B.dma
            gcount = 0

            def gather_expert(ex):
                nonlocal gcount
                hs = ex % 2
                for m in range(NM):
                    b, hf = m // 2, m % 2
                    gs = gcount % 4
                    ts = gcount % 2
                    gcount += 1
                    col = b * NE + ex
                    dma("pool", lambda e, gs=gs, hf=hf, col=col: e.indirect_dma_start(
                        out=grow[gs][:], out_offset=None, in_=h2s,
                        in_offset=bass.IndirectOffsetOnAxis(ap=idxT[:, hf, col:col + 1], axis=0)),
                        f"g{gs}", writes=[("grow", gs)])
                    for kt in range(8):
                        op("pe", lambda e, gs=gs, ts=ts, kt=kt: e.transpose(
                            out=ptr[ts][:, kt * 128:(kt + 1) * 128], in_=grow[gs][:, kt * 128:(kt + 1) * 128],
                            identity=ident_bf[:]),
                           reads=[("grow", gs)], writes=[("ptr", ts)])
                    op("act", lambda e, hs=hs, ts=ts, m=m: e.copy(
                        out=hTs[hs][:, :, m * 128:(m + 1) * 128], in_=ptr[ts][:].rearrange("p (k t) -> p k t", t=128)),
                       reads=[("ptr", ts)], writes=[("hTs", hs, m)])

            loads = [(ex, gi) for ex in range(NE) for gi in range(len(FG))]

            def issue_w13(li):
                ex, gi = loads[li]
                f0, nf = FG[gi]
                ws = li % 3
                c0, c1 = f0 * 128, (f0 + nf) * 128
                dma("pool", lambda e, ws=ws, ex=ex, c0=c0, c1=c1: e.dma_start(
                    out=w1s[ws][:, :, 0:c1 - c0], in_=w1[ex].rearrange("(kt p) f -> p kt f", p=128)[:, :, c0:c1]),
                    f"w1_{ws}", writes=[("w1s", ws)])
                dma("pool", lambda e, ws=ws, ex=ex, c0=c0, c1=c1: e.dma_start(
                    out=w3s[ws][:, :, 0:c1 - c0], in_=w3[ex].rearrange("(kt p) f -> p kt f", p=128)[:, :, c0:c1]),
                    f"w3_{ws}", writes=[("w3s", ws)])

            def issue_w2(ex):
                w2v = w2[ex].rearrange("(ft p) d -> p ft d", p=128)
                for f0 in range(0, NFT, 2):
                    dma("pool", lambda e, f0=f0, w2v=w2v: e.dma_start(out=w2s[:, f0:f0 + 2, :], in_=w2v[:, f0:f0 + 2, :]),
                        f"w2_{f0}", writes=[("w2s", f0)])

            gather_expert(0)
            issue_w13(0)
            issue_w13(1)
            li = 0
            gsl = 0
            ysl = 0
            for ex in range(NE):
                hs = ex % 2
                issue_w2(ex)
                for gi, (f0, nf) in enumerate(FG):
                    ws = li % 3
                    if li + 2 < len(loads):
                        issue_w13(li + 2)
                    li += 1
                    for fi in range(nf):
                        f = f0 + fi
                        for th in range(NTH):
                            ts_ = slice(th * NW, (th + 1) * NW)
                            HK = [("hTs", hs, m) for m in range(th * NW // 128, (th + 1) * NW // 128)]
                            g1b_, g3b_ = pg[2 * (gsl % 2)], pg[2 * (gsl % 2) + 1]
                            k1, k3 = ("pg", 2 * (gsl % 2)), ("pg", 2 * (gsl % 2) + 1)
                            sgs = gsl % 2
                            gsl += 1
                            for kt in range(8):
                                op("pe", lambda e, g1b_=g1b_, ws=ws, kt=kt, fi=fi, hs=hs, ts_=ts_: e.matmul(
                                    g1b_[:, 0:NW], lhsT=w1s[ws][:, kt, fi * 128:(fi + 1) * 128], rhs=hTs[hs][:, kt, ts_],
                                    start=(kt == 0), stop=(kt == 7)),
                                   reads=HK + [("w1s", ws)], writes=[k1])
                            for kt in range(8):
                                op("pe", lambda e, g3b_=g3b_, ws=ws, kt=kt, fi=fi, hs=hs, ts_=ts_: e.matmul(
                                    g3b_[:, 0:NW], lhsT=w3s[ws][:, kt, fi * 128:(fi + 1) * 128], rhs=hTs[hs][:, kt, ts_],
                                    start=(kt == 0), stop=(kt == 7)),
                                   reads=HK + [("w3s", ws)], writes=[k3])
                            op("act", lambda e, sgs=sgs, g1b_=g1b_: e.activation(out=sgb[sgs][:], in_=g1b_[:, 0:NW], func=AF.Silu),
                               reads=[k1], writes=[("sgb", sgs)])
                            op("dve", lambda e, sgs=sgs, g3b_=g3b_, f=f, ts_=ts_: e.tensor_tensor(
                                out=act_sb[:, f, ts_], in0=g3b_[:, 0:NW], in1=sgb[sgs][:], op=ALU.mult),
                               reads=[k3, ("sgb", sgs)], writes=[("act", f, th)])
                if ex + 1 < NE:
                    gather_expert(ex + 1)
                W2K = [("w2s", f0) for f0 in range(0, NFT, 2)]
                for m in range(NM):
                    b, hf = m // 2, m % 2
                    col = b * NE + ex
                    th = (m * 128) // NW
                    ys = ysl % 2
                    ysl += 1
                    for dh in range(2):
                        for f in range(NFT):
                            op("pe", lambda e, dh=dh, f=f, m=m: e.matmul(
                                py[dh][:, :], lhsT=act_sb[:, f, m * 128:(m + 1) * 128], rhs=w2s[:, f, dh * 512:(dh + 1) * 512],
                                start=(f == 0), stop=(f == NFT - 1)),
                               reads=[("act", f, th), ("w2s", f - f % 2)], writes=[("py", dh)])
                        if dh == 0:
                            op("act", lambda e, ys=ys, dh=dh, hf=hf, col=col: e.activation(
                                out=ye[ys][:, dh * 512:(dh + 1) * 512], in_=py[dh][:, :], func=AF.Copy,
                                scale=gateT[:, hf, col:col + 1]),
                               reads=[("py", dh)], writes=[("ye", ys, dh)])
                        else:
                            op("dve", lambda e, ys=ys, dh=dh, hf=hf, col=col: e.tensor_scalar(
                                out=ye[ys][:, dh * 512:(dh + 1) * 512], in0=py[dh][:, :],
                                scalar1=gateT[:, hf, col:col + 1], scalar2=None, op0=ALU.mult),
                               reads=[("py", dh)], writes=[("ye", ys, dh)])
                    par = ex % 2
                    dma("pool", lambda e, ys=ys, hf=hf, col=col: e.indirect_dma_start(
                        out=y, out_offset=bass.IndirectOffsetOnAxis(ap=idxT[:, hf, col:col + 1], axis=0),
                        in_=ye[ys][:], in_offset=None, compute_op=ALU.add),
                        f"sc{ys}", reads=[("ye", ys, 0), ("ye", ys, 1)] + [("ysc", 1 - par, mm) for mm in range(NM)],
                        writes=[("ysc", par, m)])
            B.emit(stB, pool)
    return nc


_NC_CACHE = {}


def _get_nc(NB):
    if NB not in _NC_CACHE:
        _NC_CACHE[NB] = build_nc(NB)
    return _NC_CACHE[NB]


def make_in_maps(inputs, NB, n_cores):
    f = lambda a: np.ascontiguousarray(np.asarray(a, dtype=np.float32))
    x = f(inputs["x"]).reshape(-1, D)
    consts = host_consts(NB)
    shared = dict(
        g1b=np.ascontiguousarray(np.broadcast_to(f(inputs["norm1_g"])[None, :], (128, D))),
        g2b=np.ascontiguousarray(np.broadcast_to(f(inputs["norm2_g"])[None, :], (128, D))),
        w_in=f(inputs["w_in"]),
        qgb=np.ascontiguousarray(np.broadcast_to(f(inputs["q_norm_g"])[None, :], (128, HD))),
        kgb=np.ascontiguousarray(np.broadcast_to(f(inputs["k_norm_g"])[None, :], (128, HD))),
        cw=np.ascontiguousarray(f(inputs["conv_dw_w"]).reshape(KC, 4, 128).transpose(2, 1, 0)),
        cb=np.ascontiguousarray(f(inputs["conv_dw_b"]).reshape(4, 128).T),
        lg=np.ascontiguousarray(f(inputs["conv_ln_g"]).reshape(4, 128).T),
        lb=np.ascontiguousarray(f(inputs["conv_ln_b"]).reshape(4, 128).T),
        w_out=f(inputs["w_out"]),
        w_router=f(inputs["w_router"]),
        w1=f(inputs["w1"]), w3=f(inputs["w3"]), w2=f(inputs["w2"]),
        **consts,
    )
    maps = []
    for c in range(n_cores):
        m = dict(shared)
        m["x"] = x[c * NB * S:(c + 1) * NB * S]
        maps.append(m)
    return maps


def kernel(**inputs):
    Bt = np.asarray(inputs["x"]).shape[0]
    NB = Bt // N_CORES
    nc = _get_nc(NB)
    in_maps = make_in_maps(inputs, NB, N_CORES)
    res = run_bass_kernel_spmd(nc, in_maps, core_ids=list(range(N_CORES)))
    out = np.concatenate([np.asarray(r["y"]) for r in res.results], axis=0)
    return out.reshape(Bt, S, D).astype(np.float32, copy=False)
```
